# Optimizing a Trainium2 kernel written in Bass

```python
import math
import jax
import jax.numpy as jnp
from jax import lax
import numpy as np

D_MODEL = 1024
BATCH = 4
SEQ = 4096
DEPTH = 2

HEAD_DIM = 64
A_HEADS = 4
MOBA_BLOCK = 256
MOBA_TOPK = 3
MOBA_Q_CHUNK = 128
B_HEADS = 8
B_KV_HEADS = 2
WINDOW = 128
C_HEADS = 4
SB_Q_BLOCK = 128
MEM_LEN = 256
X_HEADS = 4
X_HEAD_DIM = 128
D_FF = 2816
N_EXPERTS = 8
TOP_K = 2
D_FF_EXPERT = 3584

ROPE_THETA = 10000.0
EPS = 1e-6
N_BRANCH = 3

A_W = A_HEADS * HEAD_DIM
B_QW = B_HEADS * HEAD_DIM
B_KVW = B_KV_HEADS * HEAD_DIM
C_W = C_HEADS * HEAD_DIM
X_W = X_HEADS * X_HEAD_DIM
IN_W = 3 * A_W + B_QW + 2 * B_KVW + 3 * C_W + N_BRANCH * D_MODEL
N_DENSE = (DEPTH + 1) // 2
N_MOE = DEPTH // 2

kernel_name = "hybrid_moba_swa_stickbreak_gated_moe"

F32 = jnp.float32


def rms_norm(x, g):
    x32 = x.astype(F32)
    y = x32 * lax.rsqrt(jnp.mean(x32 * x32, axis=-1, keepdims=True) + EPS)
    return y.astype(x.dtype) * g


def split_heads(t, n_heads):
    b, s, _ = t.shape
    return t.reshape(b, s, n_heads, -1).transpose(0, 2, 1, 3)


def merge_heads(t):
    b, h, s, dh = t.shape
    return t.transpose(0, 2, 1, 3).reshape(b, s, h * dh)


def rope(x):
    s, dh = x.shape[2], x.shape[3]
    half = dh // 2
    inv_freq = ROPE_THETA ** (-jnp.arange(half, dtype=F32) / half)
    ang = jnp.arange(s, dtype=F32)[:, None] * inv_freq[None, :]
    cos = jnp.cos(ang).astype(x.dtype)
    sin = jnp.sin(ang).astype(x.dtype)
    x1, x2 = x[..., :half], x[..., half:]
    return jnp.concatenate([x1 * cos - x2 * sin, x2 * cos + x1 * sin], axis=-1)


def moba_attention(q, k, v):
    b, h, s, dh = q.shape
    scale = dh ** -0.5
    nb = -(-s // MOBA_BLOCK)
    pad = nb * MOBA_BLOCK - s
    kp = jnp.pad(k, ((0, 0), (0, 0), (0, pad), (0, 0))).reshape(b, h, nb, MOBA_BLOCK, dh)
    vp = jnp.pad(v, ((0, 0), (0, 0), (0, pad), (0, 0))).reshape(b, h, nb, MOBA_BLOCK, dh)
    k_mean = jnp.mean(kp.astype(F32), axis=3).astype(q.dtype)
    k_sel = min(MOBA_TOPK, nb)
    n_chunks = s // MOBA_Q_CHUNK
    qc = q.reshape(b, h, n_chunks, MOBA_Q_CHUNK, dh).transpose(2, 0, 1, 3, 4)
    offs = jnp.arange(MOBA_Q_CHUNK)
    blk_ids = jnp.arange(nb)
    bi = jnp.arange(b)[:, None, None, None]
    hi = jnp.arange(h)[None, :, None, None]

    def chunk(args):
        qi, ci = args
        t = ci * MOBA_Q_CHUNK + offs
        own = (ci * MOBA_Q_CHUNK) // MOBA_BLOCK
        gate = jnp.einsum('bhcd,bhnd->bhcn', qi, k_mean, preferred_element_type=F32)
        gate = jnp.where(blk_ids < own, gate, -jnp.inf)
        _, top_idx = lax.top_k(gate, k_sel)
        rank_ok = jnp.arange(k_sel) < own
        kg = kp[bi, hi, top_idx]
        vg = vp[bi, hi, top_idx]
        lp = jnp.einsum('bhcd,bhcnjd->bhcnj', qi, kg, preferred_element_type=F32) * scale
        lp = jnp.where(rank_ok[None, None, None, :, None], lp, -jnp.inf)
        k_own = lax.dynamic_index_in_dim(kp, own, axis=2, keepdims=False)
        v_own = lax.dynamic_index_in_dim(vp, own, axis=2, keepdims=False)
        lo = jnp.einsum('bhcd,bhjd->bhcj', qi, k_own, preferred_element_type=F32) * scale
        kpos = own * MOBA_BLOCK + jnp.arange(MOBA_BLOCK)
        lo = jnp.where(kpos[None, :] <= t[:, None], lo, -jnp.inf)
        c = qi.shape[2]
        logits = jnp.concatenate([lp.reshape(b, h, c, k_sel * MOBA_BLOCK), lo], axis=-1)
        probs = jax.nn.softmax(logits, axis=-1).astype(v.dtype)
        pp = probs[..., :k_sel * MOBA_BLOCK].reshape(b, h, c, k_sel, MOBA_BLOCK)
        po = probs[..., k_sel * MOBA_BLOCK:]
        return (jnp.einsum('bhcnj,bhcnjd->bhcd', pp, vg)
                + jnp.einsum('bhcj,bhjd->bhcd', po, v_own))

    out = lax.map(chunk, (qc, jnp.arange(n_chunks)))
    return out.transpose(1, 2, 0, 3, 4).reshape(b, h, s, dh)


def swa_sink_attention(q, k, v, sinks):
    b, hq, s, dh = q.shape
    hkv = k.shape[1]
    g = hq // hkv
    nblk = s // WINDOW
    scale = dh ** -0.5
    qb = q.reshape(b, hkv, g, nblk, WINDOW, dh)

    def band(t):
        tb = t.reshape(b, hkv, nblk, WINDOW, dh)
        prev = jnp.pad(tb, ((0, 0), (0, 0), (1, 0), (0, 0), (0, 0)))[:, :, :nblk]
        return jnp.concatenate([prev, tb], axis=3)

    kband, vband = band(k), band(v)
    logits = jnp.einsum('bkgnqd,bknjd->bkgnqj', qb, kband, preferred_element_type=F32) * scale
    qi = jnp.arange(WINDOW)[:, None] + WINDOW
    kj = jnp.arange(2 * WINDOW)[None, :]
    rel = qi - kj
    in_window = (rel >= 0) & (rel < WINDOW)
    has_prev = (jnp.arange(nblk)[:, None, None] > 0) | (kj[None] >= WINDOW)
    mask = in_window[None] & has_prev
    logits = jnp.where(mask, logits, -jnp.inf)
    sink = jnp.broadcast_to(sinks.astype(F32).reshape(1, hkv, g, 1, 1, 1), logits.shape[:-1] + (1,))
    probs = jax.nn.softmax(jnp.concatenate([logits, sink], axis=-1), axis=-1)[..., :-1]
    o = jnp.einsum('bkgnqj,bknjd->bkgnqd', probs.astype(v.dtype), vband)
    return o.reshape(b, hq, s, dh)


def stick_breaking_attention(q, k, v):
    b, h, s, dh = q.shape
    scale = dh ** -0.5
    nblk = s // SB_Q_BLOCK
    qb = q.reshape(b, h, nblk, SB_Q_BLOCK, dh).transpose(2, 0, 1, 3, 4)
    kpos = jnp.arange(s)
    offs = jnp.arange(SB_Q_BLOCK)

    def block(args):
        qi, i = args
        t = i * SB_Q_BLOCK + offs
        z = jnp.einsum('bhqd,bhkd->bhqk', qi, k, preferred_element_type=F32) * scale
        causal = kpos[None, :] < t[:, None]
        log_beta = jax.nn.log_sigmoid(z)
        log_keep = jnp.where(causal, jax.nn.log_sigmoid(-z), 0.0)
        after = lax.cumsum(log_keep, axis=3, reverse=True) - log_keep
        w = jnp.where(causal, jnp.exp(log_beta + after), 0.0)
        return jnp.einsum('bhqk,bhkd->bhqd', w.astype(v.dtype), v)

    out = lax.map(block, (qb, jnp.arange(nblk)))
    return out.transpose(1, 2, 0, 3, 4).reshape(b, h, s, dh)


def gated_mixer(h, w_in, w_proj_a, w_proj_b, w_proj_c, w_mix_out, sinks):
    proj = h @ w_in
    sizes = (A_W, A_W, A_W, B_QW, B_KVW, B_KVW, C_W, C_W, C_W)
    cuts = [int(c) for c in np.cumsum(sizes)]
    qa, ka, va, qb, kb, vb, qc, kc, vc, gates = jnp.split(proj, cuts, axis=-1)
    ya = merge_heads(moba_attention(rope(split_heads(qa, A_HEADS)),
                                    rope(split_heads(ka, A_HEADS)),
                                    split_heads(va, A_HEADS))) @ w_proj_a
    yb = merge_heads(swa_sink_attention(rope(split_heads(qb, B_HEADS)),
                                        rope(split_heads(kb, B_KV_HEADS)),
                                        split_heads(vb, B_KV_HEADS), sinks)) @ w_proj_b
    yc = merge_heads(stick_breaking_attention(split_heads(qc, C_HEADS),
                                              split_heads(kc, C_HEADS),
                                              split_heads(vc, C_HEADS))) @ w_proj_c
    ga, gb, gc = jnp.split(jax.nn.sigmoid(gates), N_BRANCH, axis=-1)
    return (ga * ya + gb * yb + gc * yc) @ w_mix_out


def memory_cross_attention(h, mem_n, w_xq, w_xkv, w_xo):
    q = split_heads(h @ w_xq, X_HEADS)
    k, v = jnp.split(mem_n @ w_xkv, 2, axis=-1)
    k = split_heads(k, X_HEADS)
    v = split_heads(v, X_HEADS)
    logits = jnp.einsum('bhsd,bhmd->bhsm', q, k, preferred_element_type=F32) * (X_HEAD_DIM ** -0.5)
    probs = jax.nn.softmax(logits, axis=-1).astype(v.dtype)
    return merge_heads(jnp.einsum('bhsm,bhmd->bhsd', probs, v)) @ w_xo


def swiglu(h, w_gate, w_up, w_down):
    return (jax.nn.silu(h @ w_gate) * (h @ w_up)) @ w_down


def moe_swiglu(h, w_router, w_gate, w_up, w_down):
    b, s, d = h.shape
    t = h.reshape(b * s, d)
    logits = (t @ w_router).astype(F32)
    top_val, top_idx = lax.top_k(logits, TOP_K)
    top_w = jax.nn.softmax(top_val, axis=-1)
    combine = jnp.sum(jax.nn.one_hot(top_idx, N_EXPERTS, dtype=F32) * top_w[..., None], axis=1)
    combine = combine.astype(t.dtype)
    out = jnp.zeros_like(t)
    for e in range(N_EXPERTS):
        out = out + combine[:, e:e + 1] * swiglu(t, w_gate[e], w_up[e], w_down[e])
    return out.reshape(b, s, d)


def setup_inputs(seed: int = 0) -> dict:
    key = jax.random.key(seed)
    ks = jax.random.split(key, 24)

    def w(k, shape, fan_in):
        return jax.random.normal(k, shape, F32) * (fan_in ** -0.5)

    def gain(k, shape):
        return 1.0 + 0.02 * jax.random.normal(k, shape, F32)

    return {
        "x": jax.random.normal(ks[0], (BATCH, SEQ, D_MODEL), F32),
        "mem": jax.random.normal(ks[1], (BATCH, MEM_LEN, D_MODEL), F32),
        "norm_mix": gain(ks[2], (DEPTH, D_MODEL)),
        "w_in": w(ks[3], (DEPTH, D_MODEL, IN_W), D_MODEL),
        "w_proj_a": w(ks[4], (DEPTH, A_W, D_MODEL), A_W),
        "w_proj_b": w(ks[5], (DEPTH, B_QW, D_MODEL), B_QW),
        "w_proj_c": w(ks[6], (DEPTH, C_W, D_MODEL), C_W),
        "w_mix_out": w(ks[7], (DEPTH, D_MODEL, D_MODEL), D_MODEL),
        "sinks": 0.5 * jax.random.normal(ks[8], (DEPTH, B_HEADS), F32),
        "norm_cross": gain(ks[9], (DEPTH, D_MODEL)),
        "norm_mem": gain(ks[10], (DEPTH, D_MODEL)),
        "w_xq": w(ks[11], (DEPTH, D_MODEL, X_W), D_MODEL),
        "w_xkv": w(ks[12], (DEPTH, D_MODEL, 2 * X_W), D_MODEL),
        "w_xo": w(ks[13], (DEPTH, X_W, D_MODEL), X_W),
        "norm_ffn": gain(ks[14], (DEPTH, D_MODEL)),
        "ffn_gate": w(ks[15], (N_DENSE, D_MODEL, D_FF), D_MODEL),
        "ffn_up": w(ks[16], (N_DENSE, D_MODEL, D_FF), D_MODEL),
        "ffn_down": w(ks[17], (N_DENSE, D_FF, D_MODEL), D_FF),
        "moe_router": w(ks[18], (N_MOE, D_MODEL, N_EXPERTS), D_MODEL),
        "moe_gate": w(ks[19], (N_MOE, N_EXPERTS, D_MODEL, D_FF_EXPERT), D_MODEL),
        "moe_up": w(ks[20], (N_MOE, N_EXPERTS, D_MODEL, D_FF_EXPERT), D_MODEL),
        "moe_down": w(ks[21], (N_MOE, N_EXPERTS, D_FF_EXPERT, D_MODEL), D_FF_EXPERT),
        "final_norm": gain(ks[22], (D_MODEL,)),
    }


def reference(x, mem, norm_mix, w_in, w_proj_a, w_proj_b, w_proj_c, w_mix_out, sinks,
              norm_cross, norm_mem, w_xq, w_xkv, w_xo, norm_ffn,
              ffn_gate, ffn_up, ffn_down, moe_router, moe_gate, moe_up, moe_down,
              final_norm):
    for l in range(DEPTH):
        h = rms_norm(x, norm_mix[l])
        x = x + gated_mixer(h, w_in[l], w_proj_a[l], w_proj_b[l], w_proj_c[l],
                            w_mix_out[l], sinks[l])
        h = rms_norm(x, norm_cross[l])
        mem_n = rms_norm(mem, norm_mem[l])
        x = x + memory_cross_attention(h, mem_n, w_xq[l], w_xkv[l], w_xo[l])
        h = rms_norm(x, norm_ffn[l])
        if l % 2 == 0:
            i = l // 2
            x = x + swiglu(h, ffn_gate[i], ffn_up[i], ffn_down[i])
        else:
            i = l // 2
            x = x + moe_swiglu(h, moe_router[i], moe_gate[i], moe_up[i], moe_down[i])
    return rms_norm(x, final_norm)
```

```python
import contextlib
import numpy as np
import ml_dtypes
import concourse.bass as bass
import concourse.mybir as mybir
from concourse.bass_utils import run_bass_kernel_spmd

F32 = mybir.dt.float32
BF16 = mybir.dt.bfloat16
U8 = mybir.dt.uint8
AF = mybir.ActivationFunctionType
ALU = mybir.AluOpType
AX = mybir.AxisListType

ENGS = ("pe", "act", "dve", "pool", "sp")
N_DMA_SEMS = 56
SAME_SYNC = {"pe": False, "act": True, "dve": True, "pool": True, "sp": False}


class Res:
    __slots__ = ("name", "w", "r", "excl")

    def __init__(self, name="", excl=False):
        self.name = name
        self.w = None
        self.r = []
        self.excl = excl


class Op:
    __slots__ = ("eng", "fn", "reads", "writes", "dma", "idx", "deps", "signal",
                 "cnt", "dsem", "dcnt", "waits")

    def __init__(self, eng, fn, reads, writes, dma):
        self.eng = eng
        self.fn = fn
        self.reads = reads
        self.writes = writes
        self.dma = dma
        self.deps = []
        self.signal = False
        self.cnt = 0
        self.dsem = -1
        self.dcnt = 0
        self.waits = []


class T:
    __slots__ = ("ap", "res")

    def __init__(self, ap, res=None, excl=False):
        self.ap = ap
        self.res = res if res is not None else Res(excl=excl)

    def __getitem__(self, k):
        return self.ap[k]


class Prog:
    def __init__(self, nc):
        self.nc = nc
        self.ops = []
        self.stack = contextlib.ExitStack()
        self.gall = Res("ALL")

    def add(self, eng, fn, reads=(), writes=(), dma=False):
        rr = [x.res if isinstance(x, T) else x for x in reads]
        ww = [x.res if isinstance(x, T) else x for x in writes]
        rr.append(self.gall)
        op = Op(eng, fn, tuple(rr), tuple(ww), dma)
        op.idx = len(self.ops)
        self.ops.append(op)
        return op

    def pe(self, fn, reads=(), writes=()):
        return self.add("pe", fn, reads, writes)

    def act(self, fn, reads=(), writes=()):
        return self.add("act", fn, reads, writes)

    def dve(self, fn, reads=(), writes=()):
        return self.add("dve", fn, reads, writes)

    def pool(self, fn, reads=(), writes=()):
        return self.add("pool", fn, reads, writes)

    def dma(self, fn, reads=(), writes=(), q="sp"):
        return self.add(q, fn, reads, writes, dma=True)

    def barrier(self, scratch_ap):
        op = Op("pool", lambda e: e.memset(scratch_ap, 0.0), (), (self.gall,), False)
        op.idx = len(self.ops)
        self.ops.append(op)


    def MM(self, out, lhsT, rhs, start, stop, reads, writes):
        return self.pe(lambda e: e.matmul(out, lhsT=lhsT, rhs=rhs, start=start, stop=stop), reads, writes)

    def ACT(self, out, in_, func, reads, writes, scale=None, bias=None, eng="act"):
        kw = {}
        if scale is not None:
            kw["scale"] = scale
        if bias is not None:
            kw["bias"] = bias
        return self.add(eng, lambda e: e.activation(out=out, in_=in_, func=func, **kw), reads, writes)

    def TT(self, out, in0, in1, op, reads, writes, eng="dve"):
        return self.add(eng, lambda e: e.tensor_tensor(out=out, in0=in0, in1=in1, op=op), reads, writes)

    def TS(self, out, in0, s1, s2, op0, op1, reads, writes, eng="dve"):
        if op1 is None:
            return self.add(eng, lambda e: e.tensor_scalar(out=out, in0=in0, scalar1=s1, scalar2=None, op0=op0), reads, writes)
        return self.add(eng, lambda e: e.tensor_scalar(out=out, in0=in0, scalar1=s1, scalar2=s2, op0=op0, op1=op1), reads, writes)

    def STT(self, out, in0, scalar, in1, op0, op1, reads, writes):
        return self.dve(lambda e: e.scalar_tensor_tensor(out=out, in0=in0, scalar=scalar, in1=in1, op0=op0, op1=op1), reads, writes)

    def COPY(self, out, in_, reads, writes, eng="dve"):
        return self.add(eng, lambda e: e.tensor_copy(out=out, in_=in_), reads, writes)

    def DMA(self, out, in_, reads=(), writes=(), q="sp"):
        return self.dma(lambda e: e.dma_start(out=out, in_=in_), reads, writes, q=q)

    def finalize(self):
        nc = self.nc
        ops = self.ops
        last_op = {}
        for op in ops:
            deps = {}
            for r in op.reads:
                if r.w is not None:
                    deps[r.w.idx] = r.w
                if r.excl:
                    for rd in r.r:
                        if rd.eng != op.eng:
                            deps[rd.idx] = rd
            for w in op.writes:
                if w.w is not None:
                    deps[w.w.idx] = w.w
                if w is self.gall:
                    for rd in w.r:
                        if rd.dma:
                            deps[rd.idx] = rd
                    for lo in last_op.values():
                        deps[lo.idx] = lo
                else:
                    for rd in w.r:
                        deps[rd.idx] = rd
            if not op.dma:
                last_op[op.eng] = op
            for r in op.reads:
                if op.dma or r is self.gall:
                    r.r.append(op)
                else:
                    for i_, rd in enumerate(r.r):
                        if (not rd.dma) and rd.eng == op.eng:
                            r.r[i_] = op
                            break
                    else:
                        r.r.append(op)
            for w in op.writes:
                w.w = op
                w.r = []
            deps.pop(op.idx, None)
            op.deps = list(deps.values())
        ndma = 0
        for op in ops:
            if op.dma:
                op.dsem = ndma % N_DMA_SEMS
                op.dcnt = 16 * (ndma // N_DMA_SEMS + 1)
                ndma += 1

        def skip(d, op):
            return (not d.dma) and (not op.dma) and d.eng == op.eng and not SAME_SYNC[d.eng]

        for op in ops:
            for d in op.deps:
                if d.dma or skip(d, op):
                    continue
                d.signal = True
        cnt = {e: 0 for e in ENGS}
        for op in ops:
            if op.dma:
                continue
            if op.signal:
                cnt[op.eng] += 1
            op.cnt = cnt[op.eng]
        waited = {e: {} for e in ENGS}
        for op in ops:
            need = {}
            for d in op.deps:
                if d.dma:
                    key = ("d", d.dsem)
                    val = d.dcnt
                else:
                    if skip(d, op):
                        continue
                    key = ("e", d.eng)
                    val = d.cnt
                if need.get(key, 0) < val:
                    need[key] = val
            if op.dma and op.dcnt > 16:
                key = ("d", op.dsem)
                val = op.dcnt - 16
                if need.get(key, 0) < val:
                    need[key] = val
            wl = []
            wd = waited[op.eng]
            for key, val in need.items():
                if wd.get(key, 0) >= val:
                    continue
                wd[key] = val
                wl.append((key, val))
            op.waits = wl
        st = self.stack
        esem = {e: st.enter_context(nc.semaphore("sem_" + e)) for e in ENGS}
        dsem = [st.enter_context(nc.semaphore("dsem%d" % i)) for i in range(min(N_DMA_SEMS, max(ndma, 1)))]
        by_eng = {e: [o for o in ops if o.eng == e] for e in ENGS}
        self.stats = {e: len(by_eng[e]) for e in ENGS}
        self.stats["waits"] = sum(len(o.waits) for o in ops)
        self.stats["signals"] = sum(1 for o in ops if o.signal)
        self.stats["ndma"] = ndma

        def emit(e, lst):
            for op in lst:
                for key, val in op.waits:
                    s = dsem[key[1]] if key[0] == "d" else esem[key[1]]
                    e.wait_ge(s, val)
                inst = op.fn(e)
                if op.dma:
                    inst.then_inc(dsem[op.dsem], 16)
                elif op.signal:
                    inst.then_inc(esem[op.eng], 1)

        block = st.enter_context(nc.Block())

        @block.tensor
        def _(e):
            emit(e, by_eng["pe"])

        @block.scalar
        def _(e):
            emit(e, by_eng["act"])

        @block.vector
        def _(e):
            emit(e, by_eng["dve"])

        @block.gpsimd
        def _(e):
            emit(e, by_eng["pool"])

        @block.sync
        def _(e):
            emit(e, by_eng["sp"])
            last = {}
            for op in ops:
                if op.dma:
                    last[op.dsem] = op.dcnt
            wd = waited["sp"]
            for s, v in last.items():
                if wd.get(("d", s), 0) < v:
                    e.wait_ge(dsem[s], v)

        st.close()


class Arena:
    def __init__(self, SB, nbytes):
        self.SB = SB
        self.nbytes = nbytes
        self.off = 0

    def alloc(self, shape, dtype, parts=None):
        esz = 2 if dtype == BF16 else 4
        parts = shape[0]
        free = list(shape[1:])
        n = 1
        for s in free:
            n *= s
        nb = (n * esz + 31) // 32 * 32
        assert self.off + nb <= self.nbytes, ("SBUF overflow", self.off, nb, self.nbytes)
        ap = self.SB[:, self.off:self.off + n * esz].bitcast(dtype)
        self.off += nb
        if len(free) == 2:
            ap = ap.rearrange("p (a b) -> p a b", b=free[1])
        elif len(free) == 3:
            ap = ap.rearrange("p (a b c) -> p a b c", b=free[1], c=free[2])
        if parts != 128:
            ap = ap[0:parts]
        return T(ap)


D = 1024
SEQ = 4096
NCTX = 4096
HALF = 2048
HD = 64
NEG = -30000.0
EPS = 1e-6
D_FF = 2816
D_FFE = 3584
NE = 8
SB_BYTES = 212480

NBLK = 13
NCOLX = NBLK * 512
C_ID, C_ONES, C_NTRI, C_NONES = 0, 128, 256, 384
C_CBC, C_CBA, C_CBB = 512, 512 + 2048, 512 + 4096
NCBF = 512 + 4096 + 256
F_ID, F_PB, F_T1, F_T0, F_GAIN, F_SINK, F_EPS, F_ONE, F_ONES32 = 0, 128, 640, 1152, 1664, 1736, 1752, 1753, 1760
NCF = 1888
G_MIX, G_CROSS, G_MEM, G_FFN, G_FINAL = 0, 16, 32, 48, 64


def _w_in_cols():
    qa, ka, va, qb, kb, vb, qc, kc, vc, gt = 0, 256, 512, 768, 1280, 1408, 1536, 1792, 2048, 2304
    sw = [(j + 32) % 64 for j in range(64)]
    cols = []

    def head(base, h, swapped=False):
        if swapped:
            return [base + 64 * h + j for j in sw]
        return [base + 64 * h + j for j in range(64)]

    def pair(base, h0):
        return head(base, h0) + head(base, h0 + 1) + head(base, h0, True) + head(base, h0 + 1, True)

    cols += pair(qa, 0) + pair(qa, 2)
    cols += pair(ka, 0) + pair(ka, 2)
    cols += pair(qb, 0) + pair(qb, 2)
    cols += pair(qb, 4) + pair(qb, 6)
    cols += pair(kb, 0)
    for h in range(4):
        cols += head(qc, h)
    for h in range(4):
        cols += head(kc, h)
    cols += list(range(va, va + 256))
    cols += list(range(vc, vc + 256))
    cols += list(range(vb, vb + 128))
    cols += [-1] * 128
    cols += list(range(gt, gt + 3072))
    assert len(cols) == NCOLX
    return np.array(cols)


def build(n_layers=2, stop_phase=None, debug=()):
    nc = bass.Bass("TRN2", target_bir_lowering=False)

    def din(name, shape, dt=F32):
        return nc.dram_tensor(name, list(shape), dt, kind="ExternalInput").ap()

    def dscr(name, shape, dt):
        return nc.dram_tensor(name, list(shape), dt, kind="Internal").ap()

    in_shapes = {
        "xT": ([D, NCTX], F32), "memT": ([D, 256], F32), "w_in_ext": ([2, D, NCOLX], F32),
        "w_proj": ([2, D, D], F32), "w_mix_out": ([2, D, D], F32), "w_xq": ([2, D, 512], F32),
        "w_xkv": ([2, D, 1024], F32), "w_xo": ([2, 512, D], F32),
        "ffn_gate": ([1, D, D_FF], F32), "ffn_up": ([1, D, D_FF], F32), "ffn_down": ([1, D_FF, D], F32),
        "moe_router": ([1, D, NE], F32), "moe_gate": ([1, NE, D, D_FFE], F32),
        "moe_up": ([1, NE, D, D_FFE], F32), "moe_down": ([1, NE, D_FFE, D], F32),
        "cbf": ([128, NCBF], BF16), "cf32": ([128, NCF], F32), "rows_bf": ([18, NCTX], BF16),
        "rope": ([4, HD, NCTX], F32),
    }
    used_inputs = {}

    def IN(name):
        if name not in used_inputs:
            shp, dt = in_shapes[name]
            used_inputs[name] = din(name, shp, dt)
        return used_inputs[name]

    out_T = nc.dram_tensor("outT", [D, HALF], F32, kind="ExternalOutput").ap()

    XT = dscr("XT", [D, NCTX], F32)
    QA = dscr("QA", [4, HD, NCTX], BF16)
    KA = dscr("KA", [4, HD, NCTX], BF16)
    QB = dscr("QB", [8, HD, NCTX], BF16)
    KB = dscr("KB", [2, HD, NCTX], BF16)
    QC = dscr("QC", [4, HD, NCTX], BF16)
    KC = dscr("KC", [4, HD, NCTX], BF16)
    VS = dscr("VS", [10, NCTX, HD], BF16)
    GS = dscr("GS", [3 * D, NCTX], BF16)
    OT = dscr("OT", [D, NCTX], BF16)
    dbg = {}
    for name, shape, dt in debug:
        dbg[name] = nc.dram_tensor(name, list(shape), dt, kind="ExternalOutput").ap()

    P = Prog(nc)
    SB = P.stack.enter_context(nc.sbuf_tensor("SB", [128, SB_BYTES], U8))
    PSB = P.stack.enter_context(nc.psum_tensor("PS", [128, 8, 512], F32))
    bank = [T(PSB[:, i, :], excl=True) for i in range(8)]
    A = Arena(SB, SB_BYTES)

    cbf = A.alloc([128, NCBF], BF16)
    cf = A.alloc([128, NCF], F32)
    bar_scr = A.alloc([128, 8], F32)
    P.DMA(cbf.ap, IN("cbf"), writes=[cbf])
    P.DMA(cf.ap, IN("cf32"), writes=[cf])
    ident = cbf[:, C_ID:C_ID + 128]
    ones_bf = cbf[:, C_ONES:C_ONES + 128]
    ntri = cbf[:, C_NTRI:C_NTRI + 128]
    nones = cbf[:, C_NONES:C_NONES + 128]
    persist_mark = A.off

    def new_phase():
        A.off = persist_mark
        P.barrier(bar_scr.ap)

    def gain_col(goff, l, c):
        return cf[:, F_GAIN + goff + 8 * l + c:F_GAIN + goff + 8 * l + c + 1]

    eps_col = cf[:, F_EPS:F_EPS + 1]

    def rmsnorm_group(xg, ntok, goff, l, out_aps, out_t, sq, rstd, nbank):
        for c in range(8):
            P.ACT(sq[:, c, 0:ntok], xg[:, c, 0:ntok], AF.Square, [xg], [sq])
        for c in range(8):
            P.MM(nbank[:, 0:ntok], ones_bf, sq[:, c, 0:ntok], c == 0, c == 7, [sq, cbf], [nbank])
        P.ACT(rstd[:, 0:ntok], nbank[:, 0:ntok], AF.Ln, [nbank, cf], [rstd], scale=1.0 / D, bias=eps_col)
        P.ACT(rstd[:, 0:ntok], rstd[:, 0:ntok], AF.Exp, [rstd], [rstd], scale=-0.5)
        for c in range(8):
            P.STT(out_aps[c], xg[:, c, 0:ntok], gain_col(goff, l, c), rstd[:, 0:ntok], ALU.mult, ALU.mult,
                  [xg, rstd, cf], [out_t])

    def load_w(dram_ap_pcn, stage, wbf, kc, ncols):
        P.DMA(stage[:, 0:kc, 0:ncols], dram_ap_pcn, writes=[stage])
        P.COPY(wbf[:, 0:kc, 0:ncols], stage[:, 0:kc, 0:ncols], [stage], [wbf])

    def phase1(l):
        new_phase()
        src = IN("xT") if l == 0 else XT
        srcv = src.rearrange("(c p) t -> p c t", p=128)
        XTv = XT.rearrange("(c p) t -> p c t", p=128)
        hT = A.alloc([128, 8, NCTX], BF16)
        mark = A.off
        xg = [A.alloc([128, 8, 512], F32) for _ in range(2)]
        sq = A.alloc([128, 8, 512], BF16)
        rstd = [A.alloc([128, 512], F32) for _ in range(2)]
        for g in range(8):
            x_ = xg[g % 2]
            gs = slice(g * 512, (g + 1) * 512)
            P.DMA(x_.ap, srcv[:, :, gs], writes=[x_])
            if l == 0:
                P.DMA(XTv[:, :, gs], x_.ap, reads=[x_])
            rmsnorm_group(x_, 512, G_MIX, l, [hT[:, c, gs] for c in range(8)], hT, sq, rstd[g % 2], bank[7])
        if stop_phase == "p1a_%d" % l:
            P.DMA(dbg["dbg_hT"].rearrange("(c p) t -> p c t", p=128), hT.ap, reads=[hT])
            return
        A.off = mark
        P.barrier(bar_scr.ap)
        stage = [A.alloc([128, 8, 512], F32) for _ in range(2)]
        wbf = [A.alloc([128, 8, 512], BF16) for _ in range(2)]
        ctab = [A.alloc([128, 512], F32) for _ in range(2)]
        stab = [A.alloc([128, 512], F32) for _ in range(2)]
        t1 = [A.alloc([128, 512], F32) for _ in range(2)]
        t2 = [A.alloc([128, 512], F32) for _ in range(2)]
        ob = [A.alloc([128, 512], BF16) for _ in range(6)]
        obi = [0]
        rope_t = IN("rope")
        wv = IN("w_in_ext")[l].rearrange("(c p) n -> p c n", p=128)
        qgroups = list(range(8)) if l == 0 else list(range(4, 8))
        kgroups = list(range(8))
        blocks = [
            (0, [("rope", "q", QA, 0, 0), ("rope", "q", QA, 2, 256)]),
            (1, [("rope", "k", KA, 0, 0), ("rope", "k", KA, 2, 256)]),
            (2, [("rope", "q", QB, 0, 0), ("rope", "q", QB, 2, 256)]),
            (3, [("rope", "q", QB, 4, 0), ("rope", "q", QB, 6, 256)]),
            (4, [("rope", "k", KB, 0, 0), ("plain", "q", QC, 0, 256), ("plain", "q", QC, 2, 384)]),
            (5, [("plain", "k", KC, 0, 0), ("plain", "k", KC, 2, 128), ("v", "k", None, 0, 256, 256)]),
            (6, [("v", "k", None, 4, 0, 384)]),
        ] + [(7 + i, [("gate", "q", None, 4 * i + j, 128 * j) for j in range(4)]) for i in range(6)]

        def issue_load(bi):
            blk = blocks[bi][0]
            load_w(wv[:, :, blk * 512:(blk + 1) * 512], stage[bi % 2], wbf[bi % 2], 8, 512)

        pb = [0]

        def nb():
            pb[0] = (pb[0] + 1) % 6
            return bank[pb[0]]

        def nob():
            o = ob[obi[0] % 6]
            obi[0] += 1
            return o

        import os
        if os.environ.get("P1_ONLY"):
            sel = [int(x) for x in os.environ["P1_ONLY"].split(",")]
            blocks = [b for b in blocks if b[0] in sel]
            A_ = None
        issue_load(0)
        for bi, (blk, jobs) in enumerate(blocks):
            if bi + 1 < len(blocks):
                issue_load(bi + 1)
            W = wbf[bi % 2]
            has_k = any(j[1] == "k" for j in jobs)
            groups = kgroups if has_k else qgroups
            for g in groups:
                gs = slice(g * 512, (g + 1) * 512)
                do_q = g in qgroups
                rope_kinds = sorted(set(j[1] for j in jobs if j[0] == "rope" and (j[1] == "k" or do_q)))
                tb = {}
                for ki, kind in enumerate(rope_kinds):
                    ti = 0 if kind == "q" else 2
                    ct, st_ = ctab[ki], stab[ki]
                    for hf_ in range(2):
                        P.DMA(ct[64 * hf_:64 * hf_ + 64, :], rope_t[ti, :, gs], writes=[ct])
                        P.DMA(st_[64 * hf_:64 * hf_ + 64, :], rope_t[ti + 1, :, gs], writes=[st_])
                    tb[kind] = (ct, st_)
                for job in jobs:
                    typ, kind = job[0], job[1]
                    if kind == "q" and not do_q:
                        continue
                    if typ == "rope":
                        _, _, dst, h, c0 = job
                        bx, by = nb(), nb()
                        for c in range(8):
                            P.MM(bx.ap, W[:, c, c0:c0 + 128], hT[:, c, gs], c == 0, c == 7, [W, hT], [bx])
                        for c in range(8):
                            P.MM(by.ap, W[:, c, c0 + 128:c0 + 256], hT[:, c, gs], c == 0, c == 7, [W, hT], [by])
                        ct, st_ = tb[kind]
                        a1, a2 = t1[obi[0] % 2], t2[obi[0] % 2]
                        o = nob()
                        P.TT(a1.ap, bx.ap, ct.ap, ALU.mult, [bx, ct], [a1])
                        P.TT(a2.ap, by.ap, st_.ap, ALU.mult, [by, st_], [a2])
                        P.TT(o.ap, a1.ap, a2.ap, ALU.add, [a1, a2], [o], eng="pool")
                        P.DMA(dst[h:h + 2, :, gs].rearrange("h d t -> (h d) t"), o.ap, reads=[o])
                    elif typ == "plain":
                        _, _, dst, h, c0 = job
                        bx = nb()
                        for c in range(8):
                            P.MM(bx.ap, W[:, c, c0:c0 + 128], hT[:, c, gs], c == 0, c == 7, [W, hT], [bx])
                        o = nob()
                        P.ACT(o.ap, bx.ap, AF.Copy, [bx], [o], scale=(0.125 if kind == "q" else 1.0))
                        P.DMA(dst[h:h + 2, :, gs].rearrange("h d t -> (h d) t"), o.ap, reads=[o])
                    elif typ == "gate":
                        _, _, _, jc, c0 = job
                        bx = nb()
                        for c in range(8):
                            P.MM(bx.ap, W[:, c, c0:c0 + 128], hT[:, c, gs], c == 0, c == 7, [W, hT], [bx])
                        o = nob()
                        P.ACT(o.ap, bx.ap, AF.Sigmoid, [bx], [o])
                        P.DMA(GS[jc * 128:(jc + 1) * 128, gs], o.ap, reads=[o])
                    elif typ == "v":
                        _, _, _, h0, c0, ncol = job
                        nh = ncol // 64
                        for tt in range(4):
                            tok = slice(g * 512 + tt * 128, g * 512 + (tt + 1) * 128)
                            bx = nb()
                            for c in range(8):
                                P.MM(bx[:, 0:ncol], hT[:, c, tok], W[:, c, c0:c0 + ncol], c == 0, c == 7, [W, hT], [bx])
                            o = nob()
                            P.ACT(o[:, 0:ncol], bx[:, 0:ncol], AF.Copy, [bx], [o])
                            P.DMA(VS[h0:h0 + nh, tok, :].rearrange("h t d -> t h d"),
                                  o[:, 0:ncol].rearrange("t (h d) -> t h d", d=64), reads=[o])


    def phase2(l):
        new_phase()
        rows = IN("rows_bf")
        q0 = 0 if l == 0 else HALF
        g0 = q0 // 512
        t0_ = q0 // 128
        Kt = [A.alloc([80, NCTX], BF16) for _ in range(2)]
        Qt = [A.alloc([80, NCTX], BF16) for _ in range(2)]
        Vt = [A.alloc([128, 32, 128], BF16) for _ in range(2)]
        dcp = [A.alloc([128, 512], F32) for _ in range(3)]
        dlo = [A.alloc([64, 512], F32) for _ in range(3)]
        esb = [A.alloc([128, 512], F32) for _ in range(2)]
        spb = [A.alloc([128, 512], BF16) for _ in range(2)]
        argb = [A.alloc([128, 512], F32) for _ in range(2)]
        wsb = [A.alloc([128, 512], BF16) for _ in range(3)]
        Rsb = A.alloc([128, 512], F32)
        rden = A.alloc([64, 512], F32)
        osb = [A.alloc([64, 512], BF16) for _ in range(3)]
        gm = A.alloc([128, 512], F32)
        usb = A.alloc([128, 512], F32)
        top8 = A.alloc([128, 32, 8], F32)
        selpad = A.alloc([128, 32, 80], BF16)
        km = A.alloc([64, 16], F32)
        kmb = A.alloc([64, 16], BF16)
        esink = A.alloc([128, 16], F32)
        cnt = {"w": 0, "o": 0, "z": 0, "r": 0, "bo": 0, "sp": 0, "o2": 0, "dc": 0, "dl": 0}

        def nxt(key, lst):
            t_ = lst[cnt[key] % len(lst)]
            cnt[key] += 1
            return t_

        def load_head(Ksrc, Qsrc, vh, slot, krows):
            K_, Q_, V_ = Kt[slot], Qt[slot], Vt[slot]
            P.DMA(K_[0:64, :], Ksrc, writes=[K_])
            P.DMA(Q_[0:64, q0:NCTX], Qsrc[:, q0:NCTX], writes=[Q_])
            for i in range(4):
                P.DMA(V_[:, 8 * i:8 * i + 8, 0:64], VS[vh, 1024 * i:1024 * (i + 1), :].rearrange("(t p) d -> p t d", p=128),
                      writes=[V_])
            return K_, Q_, V_

        def store_o(bo_, bden_, row0, gs, extra_col=None):
            if bden_ is not None:
                dc, dl = nxt("dc", dcp), nxt("dl", dlo)
                P.ACT(dc[64:128, :], bo_[64:128, :], AF.Copy, [bo_], [dc])
                P.DMA(dl.ap, dc[64:128, :], reads=[dc], writes=[dl])
                if extra_col is not None:
                    P.TS(rden.ap, dl.ap, extra_col, None, ALU.add, None, [dl, esink], [rden])
                    P.dve(lambda e: e.reciprocal(out=rden.ap, in_=rden.ap), [rden], [rden])
                else:
                    P.dve(lambda e: e.reciprocal(out=rden.ap, in_=dl.ap), [dl], [rden])
                o = nxt("o", osb)
                P.TT(o.ap, bo_[0:64, :], rden.ap, ALU.mult, [bo_, rden], [o])
            else:
                o = nxt("o", osb)
                P.ACT(o.ap, bo_[0:64, :], AF.Copy, [bo_], [o])
            P.DMA(OT[row0:row0 + 64, gs], o.ap, reads=[o])

        def run_pipeline(items, nstage):
            n = len(items)
            for i in range(n + nstage - 1):
                for s_ in range(nstage):
                    j = i - s_
                    if 0 <= j < n:
                        items[j][s_]()

        for s_ in range(2):
            P.DMA(Kt[s_][64:80, :], rows[0:16, :], writes=[Kt[s_]])
        P.pool(lambda e: e.memset(selpad.ap, 0.0), [], [selpad])
        for s_ in range(2):
            P.pool(lambda e, s_=s_: e.memset(Vt[s_][:, :, 64:128], 1.0), [], [Vt[s_]])
        nqt = (NCTX - q0) // 128

        def a_load(h):
            load_head(KA[h], QA[h], h, h % 2, 80)

        def a_sel_a(h):
            K_, Q_ = Kt[h % 2], Qt[h % 2]
            P.dve(lambda e: e.tensor_reduce(out=km.ap, in_=K_[0:64, :].rearrange("p (n j) -> p n j", j=256),
                                            axis=AX.X, op=ALU.add), [K_], [km])
            P.ACT(kmb.ap, km.ap, AF.Copy, [km], [kmb])
            bg = bank[7]
            for i in range(nqt):
                ti = t0_ + i
                P.MM(bg[:, 16 * ti:16 * ti + 16], Q_[0:64, ti * 128:(ti + 1) * 128], kmb.ap, True, True, [Q_, kmb], [bg])
            cs = slice(16 * t0_, 512)
            P.TT(gm[:, cs], bg[:, cs], cf[:, F_PB + 16 * t0_:F_PB + 512], ALU.add, [bg, cf], [gm])
            for i in range(nqt):
                ti = t0_ + i
                P.dve(lambda e, ti=ti: e.max(out=top8[:, ti, :], in_=gm[:, 16 * ti:16 * ti + 16]), [gm], [top8])
            for i in range(nqt):
                ti = t0_ + i
                P.STT(usb[:, 16 * ti:16 * ti + 16], gm[:, 16 * ti:16 * ti + 16], top8[:, ti, 2:3],
                      cf[:, F_T1 + 16 * ti:F_T1 + 16 * ti + 16], ALU.is_ge, ALU.mult, [gm, top8, cf], [usb])
            P.TT(selpad[:, t0_:32, 64:80], usb[:, cs].rearrange("p (t n) -> p t n", n=16),
                 cf[:, F_T0 + 16 * t0_:F_T0 + 512].rearrange("p (t n) -> p t n", n=16), ALU.add, [usb, cf], [selpad])

        def a_sel_b(h):
            Q_ = Qt[h % 2]
            for gi in range(nqt // 4):
                bt = bank[5 + (gi % 2)]
                for j in range(4):
                    ti = t0_ + 4 * gi + j
                    P.MM(bt[0:80, 128 * j:128 * j + 128], selpad[:, ti, :], ident, True, True, [selpad, cbf], [bt])
                P.ACT(Q_[64:80, (t0_ + 4 * gi) * 128:(t0_ + 4 * gi + 4) * 128], bt[64:80, :], AF.Copy, [bt], [Q_])

        a_load(0)
        a_sel_a(0)
        a_sel_b(0)
        items = []
        for h in range(4):
            K_, Q_, V_ = Kt[h % 2], Qt[h % 2], Vt[h % 2]
            hp = [(G, J) for G in range(g0, 8) for J in range(4 * G + 4)]
            for pi, (G, J) in enumerate(hp):
                st = {}

                def s0(h=h, pi=pi, G=G, J=J, K_=K_, Q_=Q_, st=st, n=len(hp)):
                    if pi == 4 and h + 1 < 4:
                        a_load(h + 1)
                    if pi == n // 2 and h + 1 < 4:
                        a_sel_a(h + 1)
                    if pi == n - 1 and h + 1 < 4:
                        a_sel_b(h + 1)
                    a = J - 4 * G
                    bs = bank[cnt["z"] % 3]
                    cnt["z"] += 1
                    st["bs"] = bs
                    gs = slice(G * 512, (G + 1) * 512)
                    P.MM(bs.ap, K_[0:80, J * 128:(J + 1) * 128], Q_[0:80, gs], True, a < 0, [K_, Q_], [bs])
                    if a >= 0:
                        P.MM(bs.ap, ident, cbf[:, C_CBA + a * 512:C_CBA + (a + 1) * 512], False, True, [cbf], [bs])

                def s1(st=st):
                    w_ = nxt("w", wsb)
                    st["w"] = w_
                    P.ACT(w_.ap, st["bs"].ap, AF.Exp, [st["bs"]], [w_])

                def s2(h=h, pi=pi, G=G, J=J, V_=V_, st=st, n=len(hp)):
                    nJ = 4 * G + 4
                    bo_ = bank[3 + (G % 2)]
                    w_ = st["w"]
                    P.MM(bo_.ap, V_[:, J, :], w_.ap, J == 0, J == nJ - 1, [V_, w_], [bo_])
                    if J == nJ - 1:
                        store_o(bo_, "merged", h * 64, slice(G * 512, (G + 1) * 512))

                items.append([s0, s1, s2])
        run_pipeline(items, 3)

        P.barrier(bar_scr.ap)
        for s_ in range(2):
            P.DMA(Kt[s_][64:65, :], rows[16:17, :], writes=[Kt[s_]])
            P.DMA(Qt[s_][64:65, :], rows[17:18, :], writes=[Qt[s_]])
        e2b = [A.alloc([128, 2, 512], F32) for _ in range(2)]
        sp2b = [A.alloc([128, 2, 512], BF16) for _ in range(3)]
        ar2b = [A.alloc([128, 2, 512], F32) for _ in range(2)]
        w2b = [A.alloc([128, 2, 512], BF16) for _ in range(3)]

        def c_load(h):
            load_head(KC[h], QC[h], 4 + h, h % 2, 65)

        c_load(0)
        items = []
        cz = [0]
        for h in range(4):
            K_, Q_, V_ = Kt[h % 2], Qt[h % 2], Vt[h % 2]
            hp = [(G, jp) for G in range(g0, 8) for jp in range(2 * G + 2)]
            for pi, (G, jp) in enumerate(hp):
                st = {}
                top = 4 * G + 3
                Ja, Jb = top - 2 * jp, top - 2 * jp - 1

                def s0(h=h, pi=pi, G=G, Ja=Ja, Jb=Jb, K_=K_, Q_=Q_, st=st):
                    if pi == 4 and h + 1 < 4:
                        c_load(h + 1)
                    gs = slice(G * 512, (G + 1) * 512)
                    slot = cz[0] % 2
                    cz[0] += 1
                    bA, bB = bank[2 * slot], bank[2 * slot + 1]
                    st["b"] = (bA, bB, PSB[:, 2 * slot:2 * slot + 2, :])
                    for (b_, J) in ((bA, Ja), (bB, Jb)):
                        a = J - 4 * G
                        P.MM(b_.ap, K_[0:65, J * 128:(J + 1) * 128], Q_[0:65, gs], True, False, [K_, Q_], [b_])
                        if a >= 0:
                            P.MM(b_.ap, ident, cbf[:, C_CBC + a * 512:C_CBC + (a + 1) * 512], False, False, [cbf], [b_])
                    e_ = nxt("r", e2b)
                    sp_ = nxt("sp", sp2b)
                    st["sp"] = sp_
                    P.ACT(e_.ap, st["b"][2], AF.Exp, [bA, bB], [e_])
                    P.ACT(sp_.ap, e_.ap, AF.Ln, [e_, cf], [sp_], bias=cf[:, F_ONE:F_ONE + 1])

                def s1(jp=jp, Jb=Jb, st=st):
                    bA, bB, bAB = st["b"]
                    sp_ = st["sp"]
                    P.MM(bA.ap, ntri, sp_[:, 0, :], False, True, [cbf, sp_], [bA])
                    P.MM(bB.ap, ntri, sp_[:, 1, :], False, False, [cbf, sp_], [bB])
                    P.MM(bB.ap, nones, sp_[:, 0, :], False, True, [cbf, sp_], [bB])
                    if Jb > 0:
                        br = bank[4 + (jp % 2)]
                        P.MM(br.ap, ones_bf, sp_[:, 0, :], True, False, [cbf, sp_], [br])
                        P.MM(br.ap, ones_bf, sp_[:, 1, :], False, True, [cbf, sp_], [br])
                    w_ = nxt("w", w2b)
                    st["w"] = w_
                    if jp == 0:
                        P.ACT(w_.ap, bAB, AF.Exp, [bA, bB], [w_])
                    else:
                        ar = nxt("o2", ar2b)
                        P.TT(ar[:, 0, :], bA.ap, Rsb.ap, ALU.subtract, [bA, Rsb], [ar])
                        P.TT(ar[:, 1, :], bB.ap, Rsb.ap, ALU.subtract, [bB, Rsb], [ar])
                        P.ACT(w_.ap, ar.ap, AF.Exp, [ar], [w_])
                    if Jb > 0:
                        if jp == 0:
                            P.COPY(Rsb.ap, br.ap, [br], [Rsb])
                        else:
                            P.TT(Rsb.ap, br.ap, Rsb.ap, ALU.add, [br, Rsb], [Rsb])

                def s2(h=h, G=G, jp=jp, Ja=Ja, Jb=Jb, V_=V_, st=st):
                    bo_ = bank[6 + (G % 2)]
                    w_ = st["w"]
                    P.MM(bo_[0:64, :], V_[:, Ja, 0:64], w_[:, 0, :], jp == 0, False, [V_, w_], [bo_])
                    P.MM(bo_[0:64, :], V_[:, Jb, 0:64], w_[:, 1, :], False, Jb == 0, [V_, w_], [bo_])
                    if Jb == 0:
                        store_o(bo_, None, 768 + h * 64, slice(G * 512, (G + 1) * 512))

                items.append([s0, s1, s2])
        run_pipeline(items, 3)

        P.barrier(bar_scr.ap)
        P.ACT(esink.ap, cf[:, F_SINK:F_SINK + 16], AF.Exp, [cf], [esink])

        def b_load_kv(kv):
            K_, V_ = Kt[kv], Vt[kv]
            P.DMA(K_[0:64, :], KB[kv], writes=[K_])
            for i in range(4):
                P.DMA(V_[:, 8 * i:8 * i + 8, 0:64], VS[8 + kv, 1024 * i:1024 * (i + 1), :].rearrange("(t p) d -> p t d", p=128),
                      writes=[V_])

        def b_load_q(hq):
            Q_ = Qt[hq % 2]
            P.DMA(Q_[0:64, q0:NCTX], QB[hq][:, q0:NCTX], writes=[Q_])

        b_load_kv(0)
        b_load_kv(1)
        b_load_q(0)
        items = []
        for hq in range(8):
            kv = hq // 4
            K_, V_, Q_ = Kt[kv], Vt[kv], Qt[hq % 2]
            tiles = list(range(4 * g0, 32))
            for pi, I in enumerate(tiles):
                st = {}

                def s0(hq=hq, pi=pi, I=I, K_=K_, Q_=Q_, st=st):
                    if pi == 4 and hq + 1 < 8:
                        b_load_q(hq + 1)
                    qs = slice(I * 128, (I + 1) * 128)
                    bs = bank[cnt["z"] % 3]
                    cnt["z"] += 1
                    st["bs"] = bs
                    if I > 0:
                        P.MM(bs[:, 0:128], K_[0:65, (I - 1) * 128:I * 128], Q_[0:65, qs], True, False, [K_, Q_], [bs])
                        P.MM(bs[:, 0:128], ident, cbf[:, C_CBB + 128:C_CBB + 256], False, True, [cbf], [bs])
                    P.MM(bs[:, 128:256], K_[0:65, qs], Q_[0:65, qs], True, False, [K_, Q_], [bs])
                    P.MM(bs[:, 128:256], ident, cbf[:, C_CBB:C_CBB + 128], False, True, [cbf], [bs])

                def s1(I=I, st=st):
                    w_ = nxt("w", wsb)
                    st["w"] = w_
                    c0 = 0 if I > 0 else 128
                    P.ACT(w_[:, c0:256], st["bs"][:, c0:256], AF.Exp, [st["bs"]], [w_])

                def s2(hq=hq, I=I, V_=V_, st=st):
                    G, j = I // 4, I % 4
                    bo_ = bank[3 + (G % 4)]
                    w_ = st["w"]
                    oc = slice(128 * j, 128 * j + 128)
                    if I > 0:
                        P.MM(bo_[:, oc], V_[:, I - 1, :], w_[:, 0:128], True, False, [V_, w_], [bo_])
                    P.MM(bo_[:, oc], V_[:, I, :], w_[:, 128:256], I == 0, True, [V_, w_], [bo_])
                    if j == 3:
                        store_o(bo_, "merged", 256 + hq * 64, slice(G * 512, (G + 1) * 512),
                                extra_col=esink[0:64, 8 * l + hq:8 * l + hq + 1])

                items.append([s0, s1, s2])
        run_pipeline(items, 3)

    def phase3(l):
        new_phase()
        XTv = XT.rearrange("(c p) t -> p c t", p=128)
        OTv = OT.rearrange("(c p) t -> p c t", p=128)
        GSv = GS.rearrange("(c p) t -> p c t", p=128)
        wproj = A.alloc([128, 8, 1024], BF16)
        wmix = A.alloc([128, 8, 1024], BF16)
        wxq = A.alloc([128, 8, 512], BF16)
        wxo = A.alloc([128, 4, 1024], BF16)
        KmT = A.alloc([128, 4, 256], BF16)
        Vm = A.alloc([128, 2, 512], BF16)
        mark = A.off
        stage = [A.alloc([128, 8, 512], F32) for _ in range(2)]
        wxkv = A.alloc([128, 8, 1024], BF16)
        memx = A.alloc([128, 8, 256], F32)
        memh = A.alloc([128, 8, 256], BF16)
        sq = A.alloc([128, 8, 512], BF16)
        rstd = A.alloc([128, 512], F32)
        si = [0]

        def ld(dram_pcn, dst_ap, dst_t, kc, ncols):
            st_ = stage[si[0] % 2]
            si[0] += 1
            P.DMA(st_[:, 0:kc, 0:ncols], dram_pcn, writes=[st_])
            P.COPY(dst_ap, st_[:, 0:kc, 0:ncols], [st_], [dst_t])

        wp = IN("w_proj")[l].rearrange("(c p) n -> p c n", p=128)
        wm = IN("w_mix_out")[l].rearrange("(c p) n -> p c n", p=128)
        wq = IN("w_xq")[l].rearrange("(c p) n -> p c n", p=128)
        wkv = IN("w_xkv")[l].rearrange("(c p) n -> p c n", p=128)
        wo = IN("w_xo")[l].rearrange("(c p) n -> p c n", p=128)
        for hlf in range(2):
            cs = slice(512 * hlf, 512 * hlf + 512)
            ld(wkv[:, :, cs], wxkv[:, :, cs], wxkv, 8, 512)
        for hlf in range(2):
            cs = slice(512 * hlf, 512 * hlf + 512)
            ld(wp[:, :, cs], wproj[:, :, cs], wproj, 8, 512)
            ld(wm[:, :, cs], wmix[:, :, cs], wmix, 8, 512)
            ld(wo[:, :, cs], wxo[:, :, cs], wxo, 4, 512)
        ld(wq, wxq.ap, wxq, 8, 512)
        P.DMA(memx.ap, IN("memT").rearrange("(c p) t -> p c t", p=128), writes=[memx])
        rmsnorm_group(memx, 256, G_MEM, l, [memh[:, c, :] for c in range(8)], memh, sq, rstd, bank[7])
        for hx in range(4):
            b_ = bank[hx % 2]
            for c in range(8):
                P.MM(b_[:, 0:256], wxkv[:, c, hx * 128:(hx + 1) * 128], memh[:, c, :], c == 0, c == 7, [wxkv, memh], [b_])
            P.ACT(KmT[:, hx, :], b_[:, 0:256], AF.Copy, [b_], [KmT])
        for mt in range(2):
            b_ = bank[2 + mt]
            for c in range(8):
                P.MM(b_.ap, memh[:, c, mt * 128:(mt + 1) * 128], wxkv[:, c, 512:1024], c == 0, c == 7, [wxkv, memh], [b_])
            P.ACT(Vm[:, mt, :], b_.ap, AF.Copy, [b_], [Vm])
        P.barrier(bar_scr.ap)
        A.off = mark
        xg = [A.alloc([128, 8, 512], F32) for _ in range(2)]
        ot = [A.alloc([128, 8, 512], BF16) for _ in range(2)]
        gt = A.alloc([128, 24, 512], BF16)
        mT = A.alloc([128, 8, 512], BF16)
        h2 = A.alloc([128, 8, 512], BF16)
        sq = A.alloc([128, 8, 512], BF16)
        rstd = A.alloc([128, 512], F32)
        qx = A.alloc([128, 4, 512], BF16)
        oc = A.alloc([128, 4, 512], BF16)
        pT = [A.alloc([128, 512], BF16) for _ in range(3)]
        tmpb = [A.alloc([128, 512], BF16) for _ in range(6)]
        rdn = A.alloc([128, 512], F32)
        groups = list(range(8)) if l == 0 else list(range(4, 8))
        xscale = 128 ** -0.5

        def loads(gi):
            g = groups[gi]
            gs = slice(g * 512, (g + 1) * 512)
            P.DMA(xg[gi % 2].ap, XTv[:, :, gs], writes=[xg[gi % 2]])
            P.DMA(ot[gi % 2].ap, OTv[:, :, gs], writes=[ot[gi % 2]])

        loads(0)
        pc = [0]
        for gi, g in enumerate(groups):
            gs = slice(g * 512, (g + 1) * 512)
            X, O_ = xg[gi % 2], ot[gi % 2]
            for part in range(3):
                P.DMA(gt[:, 8 * part:8 * part + 8, :], GSv[:, 8 * part:8 * part + 8, gs], writes=[gt])
            if gi + 1 < len(groups):
                loads(gi + 1)
            for j in range(8):
                js = slice(j * 128, (j + 1) * 128)
                b0, b1, b2 = bank[0 + 3 * (j % 2)], bank[1 + 3 * (j % 2)], bank[2 + 3 * (j % 2)]
                for c in range(0, 2):
                    P.MM(b0.ap, wproj[:, c, js], O_[:, c, :], c == 0, c == 1, [wproj, O_], [b0])
                for c in range(2, 6):
                    P.MM(b1.ap, wproj[:, c, js], O_[:, c, :], c == 2, c == 5, [wproj, O_], [b1])
                for c in range(6, 8):
                    P.MM(b2.ap, wproj[:, c, js], O_[:, c, :], c == 6, c == 7, [wproj, O_], [b2])
                ta, tb_, tc = tmpb[(3 * j) % 6], tmpb[(3 * j + 1) % 6], tmpb[(3 * j + 2) % 6]
                P.TT(ta.ap, b0.ap, gt[:, j, :], ALU.mult, [b0, gt], [ta])
                P.TT(tb_.ap, b1.ap, gt[:, 8 + j, :], ALU.mult, [b1, gt], [tb_])
                P.TT(tc.ap, b2.ap, gt[:, 16 + j, :], ALU.mult, [b2, gt], [tc])
                P.TT(ta.ap, ta.ap, tb_.ap, ALU.add, [ta, tb_], [ta])
                P.TT(mT[:, j, :], ta.ap, tc.ap, ALU.add, [ta, tc], [mT])
            for j in range(8):
                js = slice(j * 128, (j + 1) * 128)
                b_ = bank[j % 4]
                for c in range(8):
                    P.MM(b_.ap, wmix[:, c, js], mT[:, c, :], c == 0, c == 7, [wmix, mT], [b_])
                P.TT(X[:, j, :], b_.ap, X[:, j, :], ALU.add, [b_, X], [X])
            rmsnorm_group(X, 512, G_CROSS, l, [h2[:, c, :] for c in range(8)], h2, sq, rstd, bank[7])
            for hx in range(4):
                b_ = bank[hx % 4]
                for c in range(8):
                    P.MM(b_.ap, wxq[:, c, hx * 128:(hx + 1) * 128], h2[:, c, :], c == 0, c == 7, [wxq, h2], [b_])
                P.ACT(qx[:, hx, :], b_.ap, AF.Copy, [b_], [qx])
            for hx in range(4):
                bo_, bd_ = bank[4 + 2 * (hx % 2)], bank[5 + 2 * (hx % 2)]
                for mt in range(2):
                    bs = bank[(2 * hx + mt) % 4]
                    P.MM(bs.ap, KmT[:, hx, mt * 128:(mt + 1) * 128], qx[:, hx, :], True, True, [KmT, qx], [bs])
                    p_ = pT[pc[0] % 3]
                    pc[0] += 1
                    P.ACT(p_.ap, bs.ap, AF.Exp, [bs], [p_], scale=xscale)
                    P.MM(bo_.ap, Vm[:, mt, hx * 128:(hx + 1) * 128], p_.ap, mt == 0, mt == 1, [Vm, p_], [bo_])
                    P.MM(bd_.ap, ones_bf, p_.ap, mt == 0, mt == 1, [cbf, p_], [bd_])
                P.dve(lambda e, bd_=bd_: e.reciprocal(out=rdn.ap, in_=bd_.ap), [bd_], [rdn])
                P.TT(oc[:, hx, :], bo_.ap, rdn.ap, ALU.mult, [bo_, rdn], [oc])
            for j in range(8):
                js = slice(j * 128, (j + 1) * 128)
                b_ = bank[j % 4]
                for hx in range(4):
                    P.MM(b_.ap, wxo[:, hx, js], oc[:, hx, :], hx == 0, hx == 3, [wxo, oc], [b_])
                P.TT(X[:, j, :], b_.ap, X[:, j, :], ALU.add, [b_, X], [X])
            P.DMA(XTv[:, :, gs], X.ap, reads=[X])

    def phase4(l, last):
        new_phase()
        XTv = XT.rearrange("(c p) t -> p c t", p=128)
        moe = (l % 2 == 1)
        li = l // 2
        FB = 256
        NFC = FB // 128
        if moe:
            F_, nexp = D_FFE, NE
            wgs = [IN("moe_gate")[li, e_].rearrange("(c p) n -> p c n", p=128) for e_ in range(NE)]
            wus = [IN("moe_up")[li, e_].rearrange("(c p) n -> p c n", p=128) for e_ in range(NE)]
            wds = [IN("moe_down")[li, e_].rearrange("(c p) n -> p c n", p=128) for e_ in range(NE)]
        else:
            F_, nexp = D_FF, 1
            wgs = [IN("ffn_gate")[li].rearrange("(c p) n -> p c n", p=128)]
            wus = [IN("ffn_up")[li].rearrange("(c p) n -> p c n", p=128)]
            wds = [IN("ffn_down")[li].rearrange("(c p) n -> p c n", p=128)]
        nfb = F_ // FB
        yacc = A.alloc([128, 8, 2048], F32)
        h3 = A.alloc([128, 8, 2048], BF16)
        stage = [A.alloc([128, 8, FB], F32) for _ in range(2)]
        wg = [A.alloc([128, 8, FB], BF16) for _ in range(2)]
        wu = [A.alloc([128, 8, FB], BF16) for _ in range(2)]
        wd = [A.alloc([128, NFC, 1024], BF16) for _ in range(2)]
        aT = [A.alloc([128, NFC, 512], BF16) for _ in range(3)]
        sgs = [A.alloc([128, 512], BF16) for _ in range(2)]
        cbs = [A.alloc([128, 512], BF16) for _ in range(4)]
        sq = A.alloc([128, 8, 512], BF16)
        rstd = A.alloc([128, 512], F32)
        hf = A.alloc([128, 8, 512], F32)
        wr = A.alloc([128, 8, 8], F32)
        lg = A.alloc([128, 16, 8], F32)
        top8 = A.alloc([128, 16, 8], F32)
        rt = [A.alloc([128, 16], F32) for _ in range(3)]
        comb = A.alloc([128, 16, 8], F32)
        ctmp = A.alloc([128, 8], F32)
        dg = [A.alloc([128, 128], F32) for _ in range(2)]
        ident32 = cf[:, F_ID:F_ID + 128]
        ones32 = cf[:, F_ONES32:F_ONES32 + 128]
        sgroups = [0, 1] if l == 0 else [1]
        if moe:
            P.DMA(wr.ap, IN("moe_router")[li].rearrange("(c p) n -> p c n", p=128), writes=[wr])
        for sgi in sgroups:
            t0_ = sgi * 2048
            for g in range(4):
                P.DMA(yacc[:, :, g * 512:(g + 1) * 512], XTv[:, :, t0_ + g * 512:t0_ + (g + 1) * 512], writes=[yacc])
            for g in range(4):
                gl = slice(g * 512, (g + 1) * 512)
                xv = T(yacc.ap[:, :, gl], res=yacc.res)
                if moe:
                    rmsnorm_group(xv, 512, G_FFN, l, [hf[:, c, :] for c in range(8)], hf, sq, rstd, bank[7])
                    for c in range(8):
                        P.ACT(h3[:, c, gl], hf[:, c, :], AF.Copy, [hf], [h3])
                    for tt in range(4):
                        b_ = bank[6]
                        ti = 4 * g + tt
                        for c in range(8):
                            P.MM(b_[:, 8 * tt:8 * tt + 8], hf[:, c, tt * 128:(tt + 1) * 128], wr[:, c, :], c == 0, c == 7,
                                 [hf, wr], [b_])
                    P.COPY(lg[:, 4 * g:4 * g + 4, :], bank[6][:, 0:32].rearrange("p (t e) -> p t e", e=8), [bank[6]], [lg])
                else:
                    rmsnorm_group(xv, 512, G_FFN, l, [h3[:, c, gl] for c in range(8)], h3, sq, rstd, bank[7])
            if moe:
                for ti in range(16):
                    P.dve(lambda e, ti=ti: e.max(out=top8[:, ti, :], in_=lg[:, ti, :]), [lg], [top8])
                P.TT(rt[0].ap, top8[:, :, 1], top8[:, :, 0], ALU.subtract, [top8], [rt[0]])
                P.ACT(rt[0].ap, rt[0].ap, AF.Exp, [rt[0]], [rt[0]])
                P.TS(rt[1].ap, rt[0].ap, 1.0, None, ALU.add, None, [rt[0]], [rt[1]])
                P.dve(lambda e: e.reciprocal(out=rt[1].ap, in_=rt[1].ap), [rt[1]], [rt[1]])
                P.TT(rt[2].ap, rt[0].ap, rt[1].ap, ALU.mult, [rt[0], rt[1]], [rt[2]])
                for ti in range(16):
                    P.TS(comb[:, ti, :], lg[:, ti, :], top8[:, ti, 0:1], rt[1][:, ti:ti + 1], ALU.is_equal, ALU.mult,
                         [lg, top8, rt[1]], [comb])
                    P.TS(ctmp.ap, lg[:, ti, :], top8[:, ti, 1:2], rt[2][:, ti:ti + 1], ALU.is_equal, ALU.mult,
                         [lg, top8, rt[2]], [ctmp])
                    P.TT(comb[:, ti, :], comb[:, ti, :], ctmp.ap, ALU.add, [comb, ctmp], [comb])
            steps = [(e_, fb) for e_ in range(nexp) for fb in range(nfb)]
            sti = [0]

            def issue(k, part):
                e_, fb = steps[k]
                fs = slice(fb * FB, (fb + 1) * FB)
                (src, dst, kc, nco) = ((wgs[e_][:, :, fs], wg[k % 2], 8, FB), (wus[e_][:, :, fs], wu[k % 2], 8, FB),
                                       (wds[e_][:, NFC * fb:NFC * fb + NFC, :], wd[k % 2], NFC, 1024))[part]
                st_ = stage[sti[0] % 2]
                sti[0] += 1
                stv = T(st_.ap.rearrange("p a b -> p (a b)").rearrange("p (a b) -> p a b", b=nco), res=st_.res)
                P.DMA(stv[:, 0:kc, :], src, writes=[st_])
                P.ACT(dst.ap, stv[:, 0:kc, :], AF.Copy, [st_], [dst])

            for part_ in range(3):
                issue(0, part_)
            ai = [0]
            bdi = [0]
            bdbanks = [bank[4], bank[5], bank[6], bank[7]]
            items = []
            for k, (e_, fb) in enumerate(steps):
                for g in range(4):
                    st = {}

                    def gu(fc, k, g, st):
                        G_, U_ = wg[k % 2], wu[k % 2]
                        gl = slice(g * 512, (g + 1) * 512)
                        a_ = st["a"]
                        bg_, bu_ = bank[2 * (fc % 2)], bank[2 * (fc % 2) + 1]
                        for c in range(8):
                            P.MM(bg_.ap, G_[:, c, fc * 128:(fc + 1) * 128], h3[:, c, gl], c == 0, c == 7, [G_, h3], [bg_])
                        for c in range(8):
                            P.MM(bu_.ap, U_[:, c, fc * 128:(fc + 1) * 128], h3[:, c, gl], c == 0, c == 7, [U_, h3], [bu_])
                        s_ = sgs[fc % 2]
                        P.ACT(s_.ap, bg_.ap, AF.Silu, [bg_], [s_])
                        if moe:
                            P.TT(s_.ap, s_.ap, cbs[g].ap, ALU.mult, [s_, cbs[g]], [s_])
                        P.TT(a_[:, fc, :], bu_.ap, s_.ap, ALU.mult, [bu_, s_], [a_])

                    def s0a(k=k, e_=e_, fb=fb, g=g, st=st):
                        if g >= 1 and k + 1 < len(steps):
                            issue(k + 1, g - 1)
                        if moe and fb == 0:
                            cb_ = cbs[g]
                            for tt in range(4):
                                d_ = dg[tt % 2]
                                P.TS(d_.ap, ident32, comb[:, 4 * g + tt, e_:e_ + 1], None, ALU.mult, None, [cf, comb], [d_])
                                P.MM(bank[6][:, tt * 128:(tt + 1) * 128], ones32, d_.ap, True, True, [cf, d_], [bank[6]])
                            P.ACT(cb_.ap, bank[6].ap, AF.Copy, [bank[6]], [cb_])
                        st["a"] = aT[ai[0] % 3]
                        ai[0] += 1
                        gu(0, k, g, st)

                    def s0b(k=k, g=g, st=st):
                        for fc in range(1, NFC):
                            gu(fc, k, g, st)

                    def down(j0, j1, k, g, st):
                        D_ = wd[k % 2]
                        a_ = st["a"]
                        gl = slice(g * 512, (g + 1) * 512)
                        for j in range(j0, j1):
                            bd_ = bdbanks[bdi[0] % 4]
                            bdi[0] += 1
                            for fc in range(NFC):
                                P.MM(bd_.ap, D_[:, fc, j * 128:(j + 1) * 128], a_[:, fc, :], fc == 0, fc == NFC - 1, [D_, a_], [bd_])
                            P.TT(yacc[:, j, gl], bd_.ap, yacc[:, j, gl], ALU.add, [bd_, yacc], [yacc])

                    def s1a(k=k, g=g, st=st):
                        down(0, 4, k, g, st)

                    def s1b(k=k, g=g, st=st):
                        down(4, 8, k, g, st)

                    items.append([s0a, s0b, s1a, s1b])
            n_it = len(items)
            for i in range(n_it + 1):
                if i < n_it:
                    items[i][0]()
                if i >= 1:
                    items[i - 1][2]()
                if i < n_it:
                    items[i][1]()
                if i >= 1:
                    items[i - 1][3]()
            if not last:
                for g in range(4):
                    P.DMA(XTv[:, :, t0_ + g * 512:t0_ + (g + 1) * 512], yacc[:, :, g * 512:(g + 1) * 512], reads=[yacc])
            else:
                oT = out_T.rearrange("(c p) t -> p c t", p=128)
                for g in range(4):
                    gl = slice(g * 512, (g + 1) * 512)
                    xv = T(yacc.ap[:, :, gl], res=yacc.res)
                    rmsnorm_group(xv, 512, G_FINAL, 0, [hf[:, c, :] for c in range(8)], hf, sq, rstd, bank[7])
                    P.DMA(oT[:, :, gl], hf.ap, reads=[hf])

    cbs_cur = [None] * 4

    phases = []
    for l in range(n_layers):
        phases.append(("p1_%d" % l, lambda l=l: phase1(l)))
        phases.append(("p2_%d" % l, lambda l=l: phase2(l)))
        phases.append(("p3_%d" % l, lambda l=l: phase3(l)))
        phases.append(("p4_%d" % l, lambda l=l: phase4(l, l == n_layers - 1 and n_layers == 2)))
    for name, fn in phases:
        fn()
        if stop_phase is not None and stop_phase.replace("p1a", "p1") == name:
            break
    if dbg:
        new_phase()
        srcs = {"QA": QA, "KA": KA, "QB": QB, "KB": KB, "QC": QC, "KC": KC, "VS": VS, "GS": GS, "OT": OT, "XT": XT}
        for name, ap in dbg.items():
            if name.replace("dbg_", "") not in srcs:
                continue
            s = srcs[name.replace("dbg_", "")]
            P.DMA(ap, s)
    P.finalize()
    P.used_inputs = list(used_inputs.keys())
    return nc, P


def _tables(role):
    bf = ml_dtypes.bfloat16
    cb = np.zeros((128, NCBF), np.float32)
    cb[:, C_ID:C_ID + 128] = np.eye(128)
    cb[:, C_ONES:C_ONES + 128] = 1.0
    j = np.arange(128)[:, None]
    s_ = np.arange(128)[None, :]
    cb[:, C_NTRI:C_NTRI + 128] = np.where(j >= s_, -1.0, 0.0)
    cb[:, C_NONES:C_NONES + 128] = -1.0
    k = np.arange(128)[:, None]
    q = np.arange(128)[None, :]
    for a in range(4):
        for c in range(4):
            strict = np.where((a > c) | ((a == c) & (k >= q)), NEG, 0.0)
            nonstrict = np.where((a > c) | ((a == c) & (k > q)), NEG, 0.0)
            cb[:, C_CBC + a * 512 + c * 128:C_CBC + a * 512 + (c + 1) * 128] = strict
            cb[:, C_CBA + a * 512 + c * 128:C_CBA + a * 512 + (c + 1) * 128] = nonstrict
    cb[:, C_CBB:C_CBB + 128] = np.where(k > q, NEG, 0.0)
    cb[:, C_CBB + 128:C_CBB + 256] = np.where(k <= q, NEG, 0.0)
    cbf = cb.astype(bf)
    pbt = np.zeros((16, 16), np.float32)
    t1 = np.zeros((16, 16), np.float32)
    t0 = np.zeros((16, 16), np.float32)
    for own in range(16):
        for n in range(16):
            if role == 1 or own < 8:
                valid = n < own
            else:
                valid = 8 <= n < own
            pbt[own, n] = 0.0 if valid else -1e30
            t1[own, n] = -NEG if valid else 0.0
            t0[own, n] = NEG if valid else (0.0 if n == own else NEG)
    rows = np.zeros((18, NCTX), np.float32)
    pos = np.arange(NCTX)
    rows[np.arange(NCTX) // 256, np.arange(NCTX)] = 1.0
    if role == 0:
        rows[16, :HALF] = NEG
        rows[17, HALF:] = 1.0
        pos = np.where(pos >= HALF, pos - HALF, pos)
    else:
        rows[17, :] = 1.0
    rows_bf = rows.astype(bf)
    half = 32
    inv_freq = (np.float32(10000.0) ** (-np.arange(half, dtype=np.float32) / np.float32(half))).astype(np.float32)
    ang = pos.astype(np.float32)[:, None] * inv_freq[None, :]
    cos = np.cos(ang).astype(np.float32).T
    sin = np.sin(ang).astype(np.float32).T
    cosT = np.concatenate([cos, cos], 0)
    sinT = np.concatenate([-sin, sin], 0)
    rope = np.stack([cosT * np.float32(0.125), sinT * np.float32(0.125), cosT, sinT]).astype(np.float32)
    return cbf, pbt, t1, t0, rows_bf, rope


def _prep_inputs(inp):
    f = lambda a: np.ascontiguousarray(np.asarray(a, dtype=np.float32))
    x = f(inp["x"])
    mem = f(inp["mem"])
    cols = _w_in_cols()
    w_in = f(inp["w_in"])
    w_in_ext = np.zeros((2, D, NCOLX), np.float32)
    valid = cols >= 0
    w_in_ext[:, :, valid] = w_in[:, :, cols[valid]]
    w_proj = np.concatenate([f(inp["w_proj_a"]), f(inp["w_proj_b"]), f(inp["w_proj_c"])], axis=1)
    gains = np.zeros((128, 72), np.float32)
    for off, key in ((G_MIX, "norm_mix"), (G_CROSS, "norm_cross"), (G_MEM, "norm_mem"), (G_FFN, "norm_ffn")):
        g = f(inp[key])
        for l in range(2):
            gains[:, off + 8 * l:off + 8 * l + 8] = g[l].reshape(8, 128).T
    gains[:, G_FINAL:G_FINAL + 8] = f(inp["final_norm"]).reshape(8, 128).T
    sinks = f(inp["sinks"]).reshape(1, 16)
    shared = {
        "w_in_ext": w_in_ext, "w_proj": w_proj, "w_mix_out": f(inp["w_mix_out"]),
        "w_xq": f(inp["w_xq"]), "w_xkv": f(inp["w_xkv"]), "w_xo": f(inp["w_xo"]),
        "ffn_gate": f(inp["ffn_gate"]), "ffn_up": f(inp["ffn_up"]), "ffn_down": f(inp["ffn_down"]),
        "moe_router": f(inp["moe_router"]), "moe_gate": f(inp["moe_gate"]), "moe_up": f(inp["moe_up"]),
        "moe_down": f(inp["moe_down"]),
    }
    in_maps = []
    tabs = [_tables(0), _tables(1)]
    for c in range(8):
        b, role = c // 2, c % 2
        cbf, pbt, t1, t0, rows_bf, rope = tabs[role]
        if role == 1:
            ctx = x[b]
        else:
            ctx = np.concatenate([np.zeros((HALF, D), np.float32), x[b, :HALF]], axis=0)
        cf = np.zeros((128, NCF), np.float32)
        cf[:, F_ID:F_ID + 128] = np.eye(128, dtype=np.float32)
        own_of_tile = np.arange(32) // 2
        cf[:, F_PB:F_PB + 512] = pbt[own_of_tile].reshape(1, 512)
        cf[:, F_T1:F_T1 + 512] = t1[own_of_tile].reshape(1, 512)
        cf[:, F_T0:F_T0 + 512] = t0[own_of_tile].reshape(1, 512)
        cf[:, F_ONE] = 1.0
        cf[:, F_ONES32:F_ONES32 + 128] = 1.0
        cf[:, F_GAIN:F_GAIN + 72] = gains
        cf[:, F_SINK:F_SINK + 16] = sinks
        cf[:, F_EPS] = EPS
        m = dict(shared)
        m.update({"xT": np.ascontiguousarray(ctx.T), "memT": np.ascontiguousarray(mem[b].T),
                  "cbf": cbf, "cf32": cf, "rows_bf": rows_bf, "rope": rope})
        in_maps.append(m)
    return in_maps


_NC_CACHE = {}


def kernel(**inputs):
    in_maps = _prep_inputs(inputs)
    if "nc" not in _NC_CACHE:
        _NC_CACHE["nc"] = build()[0]
    nc = _NC_CACHE["nc"]
    res = run_bass_kernel_spmd(nc, in_maps, core_ids=list(range(8)))
    out = np.zeros((4, SEQ, D), np.float32)
    for c in range(8):
        b, role = c // 2, c % 2
        o = np.asarray(res.results[c]["outT"]).T
        if role == 0:
            out[b, :HALF] = o
        else:
            out[b, HALF:] = o
    return out
```

```python
import contextlib
import numpy as np
import ml_dtypes
import concourse.bass as bass
import concourse.mybir as mybir
from concourse.bass_utils import run_bass_kernel_spmd

F32 = mybir.dt.float32
BF16 = mybir.dt.bfloat16
U8 = mybir.dt.uint8
AF = mybir.ActivationFunctionType
ALU = mybir.AluOpType
AX = mybir.AxisListType

ENGS = ("pe", "act", "dve", "pool", "sp")
N_DMA_SEMS = 56
SAME_SYNC = {"pe": False, "act": True, "dve": True, "pool": True, "sp": False}


class Res:
    __slots__ = ("name", "w", "r", "excl")

    def __init__(self, name="", excl=False):
        self.name = name
        self.w = None
        self.r = []
        self.excl = excl


class Op:
    __slots__ = ("eng", "fn", "reads", "writes", "dma", "idx", "deps", "signal",
                 "cnt", "dsem", "dcnt", "waits")

    def __init__(self, eng, fn, reads, writes, dma):
        self.eng = eng
        self.fn = fn
        self.reads = reads
        self.writes = writes
        self.dma = dma
        self.deps = []
        self.signal = False
        self.cnt = 0
        self.dsem = -1
        self.dcnt = 0
        self.waits = []


class T:
    __slots__ = ("ap", "res")

    def __init__(self, ap, res=None, excl=False):
        self.ap = ap
        self.res = res if res is not None else Res(excl=excl)

    def __getitem__(self, k):
        return self.ap[k]


class Prog:
    def __init__(self, nc):
        self.nc = nc
        self.ops = []
        self.stack = contextlib.ExitStack()
        self.gall = Res("ALL")

    def add(self, eng, fn, reads=(), writes=(), dma=False):
        rr = [x.res if isinstance(x, T) else x for x in reads]
        ww = [x.res if isinstance(x, T) else x for x in writes]
        rr.append(self.gall)
        op = Op(eng, fn, tuple(rr), tuple(ww), dma)
        op.idx = len(self.ops)
        self.ops.append(op)
        return op

    def pe(self, fn, reads=(), writes=()):
        return self.add("pe", fn, reads, writes)

    def act(self, fn, reads=(), writes=()):
        return self.add("act", fn, reads, writes)

    def dve(self, fn, reads=(), writes=()):
        return self.add("dve", fn, reads, writes)

    def pool(self, fn, reads=(), writes=()):
        return self.add("pool", fn, reads, writes)

    def dma(self, fn, reads=(), writes=(), q="sp"):
        return self.add(q, fn, reads, writes, dma=True)

    def barrier(self, scratch_ap):
        op = Op("pool", lambda e: e.memset(scratch_ap, 0.0), (), (self.gall,), False)
        op.idx = len(self.ops)
        self.ops.append(op)


    def MM(self, out, lhsT, rhs, start, stop, reads, writes):
        return self.pe(lambda e: e.matmul(out, lhsT=lhsT, rhs=rhs, start=start, stop=stop), reads, writes)

    def ACT(self, out, in_, func, reads, writes, scale=None, bias=None, eng="act"):
        kw = {}
        if scale is not None:
            kw["scale"] = scale
        if bias is not None:
            kw["bias"] = bias
        return self.add(eng, lambda e: e.activation(out=out, in_=in_, func=func, **kw), reads, writes)

    def TT(self, out, in0, in1, op, reads, writes, eng="dve"):
        return self.add(eng, lambda e: e.tensor_tensor(out=out, in0=in0, in1=in1, op=op), reads, writes)

    def TS(self, out, in0, s1, s2, op0, op1, reads, writes, eng="dve"):
        if op1 is None:
            return self.add(eng, lambda e: e.tensor_scalar(out=out, in0=in0, scalar1=s1, scalar2=None, op0=op0), reads, writes)
        return self.add(eng, lambda e: e.tensor_scalar(out=out, in0=in0, scalar1=s1, scalar2=s2, op0=op0, op1=op1), reads, writes)

    def STT(self, out, in0, scalar, in1, op0, op1, reads, writes):
        return self.dve(lambda e: e.scalar_tensor_tensor(out=out, in0=in0, scalar=scalar, in1=in1, op0=op0, op1=op1), reads, writes)

    def COPY(self, out, in_, reads, writes, eng="dve"):
        return self.add(eng, lambda e: e.tensor_copy(out=out, in_=in_), reads, writes)

    def DMA(self, out, in_, reads=(), writes=(), q="sp"):
        return self.dma(lambda e: e.dma_start(out=out, in_=in_), reads, writes, q=q)

    def finalize(self):
        nc = self.nc
        ops = self.ops
        last_op = {}
        for op in ops:
            deps = {}
            for r in op.reads:
                if r.w is not None:
                    deps[r.w.idx] = r.w
                if r.excl:
                    for rd in r.r:
                        if rd.eng != op.eng:
                            deps[rd.idx] = rd
            for w in op.writes:
                if w.w is not None:
                    deps[w.w.idx] = w.w
                if w is self.gall:
                    for rd in w.r:
                        if rd.dma:
                            deps[rd.idx] = rd
                    for lo in last_op.values():
                        deps[lo.idx] = lo
                else:
                    for rd in w.r:
                        deps[rd.idx] = rd
            if not op.dma:
                last_op[op.eng] = op
            for r in op.reads:
                if op.dma or r is self.gall:
                    r.r.append(op)
                else:
                    for i_, rd in enumerate(r.r):
                        if (not rd.dma) and rd.eng == op.eng:
                            r.r[i_] = op
                            break
                    else:
                        r.r.append(op)
            for w in op.writes:
                w.w = op
                w.r = []
            deps.pop(op.idx, None)
            op.deps = list(deps.values())
        ndma = 0
        for op in ops:
            if op.dma:
                op.dsem = ndma % N_DMA_SEMS
                op.dcnt = 16 * (ndma // N_DMA_SEMS + 1)
                ndma += 1

        def skip(d, op):
            return (not d.dma) and (not op.dma) and d.eng == op.eng and not SAME_SYNC[d.eng]

        for op in ops:
            for d in op.deps:
                if d.dma or skip(d, op):
                    continue
                d.signal = True
        cnt = {e: 0 for e in ENGS}
        for op in ops:
            if op.dma:
                continue
            if op.signal:
                cnt[op.eng] += 1
            op.cnt = cnt[op.eng]
        waited = {e: {} for e in ENGS}
        for op in ops:
            need = {}
            for d in op.deps:
                if d.dma:
                    key = ("d", d.dsem)
                    val = d.dcnt
                else:
                    if skip(d, op):
                        continue
                    key = ("e", d.eng)
                    val = d.cnt
                if need.get(key, 0) < val:
                    need[key] = val
            if op.dma and op.dcnt > 16:
                key = ("d", op.dsem)
                val = op.dcnt - 16
                if need.get(key, 0) < val:
                    need[key] = val
            wl = []
            wd = waited[op.eng]
            for key, val in need.items():
                if wd.get(key, 0) >= val:
                    continue
                wd[key] = val
                wl.append((key, val))
            op.waits = wl
        st = self.stack
        esem = {e: st.enter_context(nc.semaphore("sem_" + e)) for e in ENGS}
        dsem = [st.enter_context(nc.semaphore("dsem%d" % i)) for i in range(min(N_DMA_SEMS, max(ndma, 1)))]
        by_eng = {e: [o for o in ops if o.eng == e] for e in ENGS}
        self.stats = {e: len(by_eng[e]) for e in ENGS}
        self.stats["waits"] = sum(len(o.waits) for o in ops)
        self.stats["signals"] = sum(1 for o in ops if o.signal)
        self.stats["ndma"] = ndma

        def emit(e, lst):
            for op in lst:
                for key, val in op.waits:
                    s = dsem[key[1]] if key[0] == "d" else esem[key[1]]
                    e.wait_ge(s, val)
                inst = op.fn(e)
                if op.dma:
                    inst.then_inc(dsem[op.dsem], 16)
                elif op.signal:
                    inst.then_inc(esem[op.eng], 1)

        block = st.enter_context(nc.Block())

        @block.tensor
        def _(e):
            emit(e, by_eng["pe"])

        @block.scalar
        def _(e):
            emit(e, by_eng["act"])

        @block.vector
        def _(e):
            emit(e, by_eng["dve"])

        @block.gpsimd
        def _(e):
            emit(e, by_eng["pool"])

        @block.sync
        def _(e):
            emit(e, by_eng["sp"])
            last = {}
            for op in ops:
                if op.dma:
                    last[op.dsem] = op.dcnt
            wd = waited["sp"]
            for s, v in last.items():
                if wd.get(("d", s), 0) < v:
                    e.wait_ge(dsem[s], v)

        st.close()


class Arena:
    def __init__(self, SB, nbytes):
        self.SB = SB
        self.nbytes = nbytes
        self.off = 0

    def alloc(self, shape, dtype, parts=None):
        esz = 2 if dtype == BF16 else 4
        parts = shape[0]
        free = list(shape[1:])
        n = 1
        for s in free:
            n *= s
        nb = (n * esz + 31) // 32 * 32
        assert self.off + nb <= self.nbytes, ("SBUF overflow", self.off, nb, self.nbytes)
        ap = self.SB[:, self.off:self.off + n * esz].bitcast(dtype)
        self.off += nb
        if len(free) == 2:
            ap = ap.rearrange("p (a b) -> p a b", b=free[1])
        elif len(free) == 3:
            ap = ap.rearrange("p (a b c) -> p a b c", b=free[1], c=free[2])
        if parts != 128:
            ap = ap[0:parts]
        return T(ap)


D = 1024
SEQ = 4096
NCTX = 4096
HALF = 2048
HD = 64
NEG = -30000.0
EPS = 1e-6
D_FF = 2816
D_FFE = 3584
NE = 8
SB_BYTES = 212480

NBLK = 13
NCOLX = NBLK * 512
C_ID, C_ONES, C_NTRI, C_NONES = 0, 128, 256, 384
C_CBC, C_CBA, C_CBB = 512, 512 + 2048, 512 + 4096
NCBF = 512 + 4096 + 256
F_ID, F_PB, F_T1, F_T0, F_GAIN, F_SINK, F_EPS, F_ONE, F_ONES32 = 0, 128, 640, 1152, 1664, 1736, 1752, 1753, 1760
NCF = 1888
G_MIX, G_CROSS, G_MEM, G_FFN, G_FINAL = 0, 16, 32, 48, 64


def _w_in_cols():
    qa, ka, va, qb, kb, vb, qc, kc, vc, gt = 0, 256, 512, 768, 1280, 1408, 1536, 1792, 2048, 2304
    sw = [(j + 32) % 64 for j in range(64)]
    cols = []

    def head(base, h, swapped=False):
        if swapped:
            return [base + 64 * h + j for j in sw]
        return [base + 64 * h + j for j in range(64)]

    def pair(base, h0):
        return head(base, h0) + head(base, h0 + 1) + head(base, h0, True) + head(base, h0 + 1, True)

    cols += pair(qa, 0) + pair(qa, 2)
    cols += pair(ka, 0) + pair(ka, 2)
    cols += pair(qb, 0) + pair(qb, 2)
    cols += pair(qb, 4) + pair(qb, 6)
    cols += pair(kb, 0)
    for h in range(4):
        cols += head(qc, h)
    for h in range(4):
        cols += head(kc, h)
    cols += list(range(va, va + 256))
    cols += list(range(vc, vc + 256))
    cols += list(range(vb, vb + 128))
    cols += [-1] * 128
    cols += list(range(gt, gt + 3072))
    assert len(cols) == NCOLX
    return np.array(cols)


def build(n_layers=2, stop_phase=None, debug=()):
    nc = bass.Bass("TRN2", target_bir_lowering=False)

    def din(name, shape, dt=F32):
        return nc.dram_tensor(name, list(shape), dt, kind="ExternalInput").ap()

    def dscr(name, shape, dt):
        return nc.dram_tensor(name, list(shape), dt, kind="Internal").ap()

    in_shapes = {
        "xT": ([D, NCTX], F32), "memT": ([D, 256], F32), "w_in_ext": ([2, D, NCOLX], F32),
        "w_proj": ([2, D, D], F32), "w_mix_out": ([2, D, D], F32), "w_xq": ([2, D, 512], F32),
        "w_xkv": ([2, D, 1024], F32), "w_xo": ([2, 512, D], F32),
        "ffn_gate": ([1, D, D_FF], F32), "ffn_up": ([1, D, D_FF], F32), "ffn_down": ([1, D_FF, D], F32),
        "moe_router": ([1, D, NE], F32), "moe_gate": ([1, NE, D, D_FFE], F32),
        "moe_up": ([1, NE, D, D_FFE], F32), "moe_down": ([1, NE, D_FFE, D], F32),
        "cbf": ([128, NCBF], BF16), "cf32": ([128, NCF], F32), "rows_bf": ([18, NCTX], BF16),
        "rope": ([4, HD, NCTX], F32),
    }
    used_inputs = {}

    def IN(name):
        if name not in used_inputs:
            shp, dt = in_shapes[name]
            used_inputs[name] = din(name, shp, dt)
        return used_inputs[name]

    out_T = nc.dram_tensor("outT", [D, HALF], F32, kind="ExternalOutput").ap()

    XT = dscr("XT", [D, NCTX], F32)
    QA = dscr("QA", [4, HD, NCTX], BF16)
    KA = dscr("KA", [4, HD, NCTX], BF16)
    QB = dscr("QB", [8, HD, NCTX], BF16)
    KB = dscr("KB", [2, HD, NCTX], BF16)
    QC = dscr("QC", [4, HD, NCTX], BF16)
    KC = dscr("KC", [4, HD, NCTX], BF16)
    VS = dscr("VS", [10, NCTX, HD], BF16)
    GS = dscr("GS", [3 * D, NCTX], BF16)
    OT = dscr("OT", [D, NCTX], BF16)
    dbg = {}
    for name, shape, dt in debug:
        dbg[name] = nc.dram_tensor(name, list(shape), dt, kind="ExternalOutput").ap()

    P = Prog(nc)
    SB = P.stack.enter_context(nc.sbuf_tensor("SB", [128, SB_BYTES], U8))
    PSB = P.stack.enter_context(nc.psum_tensor("PS", [128, 8, 512], F32))
    bank = [T(PSB[:, i, :], excl=True) for i in range(8)]
    A = Arena(SB, SB_BYTES)

    cbf = A.alloc([128, NCBF], BF16)
    cf = A.alloc([128, NCF], F32)
    bar_scr = A.alloc([128, 8], F32)
    P.DMA(cbf.ap, IN("cbf"), writes=[cbf])
    P.DMA(cf.ap, IN("cf32"), writes=[cf])
    ident = cbf[:, C_ID:C_ID + 128]
    ones_bf = cbf[:, C_ONES:C_ONES + 128]
    ntri = cbf[:, C_NTRI:C_NTRI + 128]
    nones = cbf[:, C_NONES:C_NONES + 128]
    persist_mark = A.off

    def new_phase():
        A.off = persist_mark
        P.barrier(bar_scr.ap)

    def gain_col(goff, l, c):
        return cf[:, F_GAIN + goff + 8 * l + c:F_GAIN + goff + 8 * l + c + 1]

    eps_col = cf[:, F_EPS:F_EPS + 1]

    def rmsnorm_group(xg, ntok, goff, l, out_aps, out_t, sq, rstd, nbank):
        for c in range(8):
            P.ACT(sq[:, c, 0:ntok], xg[:, c, 0:ntok], AF.Square, [xg], [sq])
        for c in range(8):
            P.MM(nbank[:, 0:ntok], ones_bf, sq[:, c, 0:ntok], c == 0, c == 7, [sq, cbf], [nbank])
        P.ACT(rstd[:, 0:ntok], nbank[:, 0:ntok], AF.Ln, [nbank, cf], [rstd], scale=1.0 / D, bias=eps_col)
        P.ACT(rstd[:, 0:ntok], rstd[:, 0:ntok], AF.Exp, [rstd], [rstd], scale=-0.5)
        for c in range(8):
            P.STT(out_aps[c], xg[:, c, 0:ntok], gain_col(goff, l, c), rstd[:, 0:ntok], ALU.mult, ALU.mult,
                  [xg, rstd, cf], [out_t])

    def load_w(dram_ap_pcn, stage, wbf, kc, ncols):
        P.DMA(stage[:, 0:kc, 0:ncols], dram_ap_pcn, writes=[stage])
        P.COPY(wbf[:, 0:kc, 0:ncols], stage[:, 0:kc, 0:ncols], [stage], [wbf])

    def phase1(l):
        new_phase()
        src = IN("xT") if l == 0 else XT
        srcv = src.rearrange("(c p) t -> p c t", p=128)
        XTv = XT.rearrange("(c p) t -> p c t", p=128)
        hT = A.alloc([128, 8, NCTX], BF16)
        mark = A.off
        xg = [A.alloc([128, 8, 512], F32) for _ in range(2)]
        sq = A.alloc([128, 8, 512], BF16)
        rstd = [A.alloc([128, 512], F32) for _ in range(2)]
        for g in range(8):
            x_ = xg[g % 2]
            gs = slice(g * 512, (g + 1) * 512)
            P.DMA(x_.ap, srcv[:, :, gs], writes=[x_])
            if l == 0:
                P.DMA(XTv[:, :, gs], x_.ap, reads=[x_])
            rmsnorm_group(x_, 512, G_MIX, l, [hT[:, c, gs] for c in range(8)], hT, sq, rstd[g % 2], bank[7])
        if stop_phase == "p1a_%d" % l:
            P.DMA(dbg["dbg_hT"].rearrange("(c p) t -> p c t", p=128), hT.ap, reads=[hT])
            return
        A.off = mark
        P.barrier(bar_scr.ap)
        stage = [A.alloc([128, 8, 512], F32) for _ in range(2)]
        wbf = [A.alloc([128, 8, 512], BF16) for _ in range(2)]
        ctab = [A.alloc([128, 512], F32) for _ in range(2)]
        stab = [A.alloc([128, 512], F32) for _ in range(2)]
        t1 = [A.alloc([128, 512], F32) for _ in range(2)]
        t2 = [A.alloc([128, 512], F32) for _ in range(2)]
        ob = [A.alloc([128, 512], BF16) for _ in range(6)]
        obi = [0]
        rope_t = IN("rope")
        wv = IN("w_in_ext")[l].rearrange("(c p) n -> p c n", p=128)
        qgroups = list(range(8)) if l == 0 else list(range(4, 8))
        kgroups = list(range(8))
        blocks = [
            (0, [("rope", "q", QA, 0, 0), ("rope", "q", QA, 2, 256)]),
            (1, [("rope", "k", KA, 0, 0), ("rope", "k", KA, 2, 256)]),
            (2, [("rope", "q", QB, 0, 0), ("rope", "q", QB, 2, 256)]),
            (3, [("rope", "q", QB, 4, 0), ("rope", "q", QB, 6, 256)]),
            (4, [("rope", "k", KB, 0, 0), ("plain", "q", QC, 0, 256), ("plain", "q", QC, 2, 384)]),
            (5, [("plain", "k", KC, 0, 0), ("plain", "k", KC, 2, 128), ("v", "k", None, 0, 256, 256)]),
            (6, [("v", "k", None, 4, 0, 384)]),
        ] + [(7 + i, [("gate", "q", None, 4 * i + j, 128 * j) for j in range(4)]) for i in range(6)]

        def issue_load(bi):
            blk = blocks[bi][0]
            load_w(wv[:, :, blk * 512:(blk + 1) * 512], stage[bi % 2], wbf[bi % 2], 8, 512)

        pb = [0]

        def nb():
            pb[0] = (pb[0] + 1) % 6
            return bank[pb[0]]

        def nob():
            o = ob[obi[0] % 6]
            obi[0] += 1
            return o

        import os
        if os.environ.get("P1_ONLY"):
            sel = [int(x) for x in os.environ["P1_ONLY"].split(",")]
            blocks = [b for b in blocks if b[0] in sel]
            A_ = None
        issue_load(0)
        for bi, (blk, jobs) in enumerate(blocks):
            if bi + 1 < len(blocks):
                issue_load(bi + 1)
            W = wbf[bi % 2]
            has_k = any(j[1] == "k" for j in jobs)
            groups = kgroups if has_k else qgroups
            for g in groups:
                gs = slice(g * 512, (g + 1) * 512)
                do_q = g in qgroups
                rope_kinds = sorted(set(j[1] for j in jobs if j[0] == "rope" and (j[1] == "k" or do_q)))
                tb = {}
                for ki, kind in enumerate(rope_kinds):
                    ti = 0 if kind == "q" else 2
                    ct, st_ = ctab[ki], stab[ki]
                    for hf_ in range(2):
                        P.DMA(ct[64 * hf_:64 * hf_ + 64, :], rope_t[ti, :, gs], writes=[ct])
                        P.DMA(st_[64 * hf_:64 * hf_ + 64, :], rope_t[ti + 1, :, gs], writes=[st_])
                    tb[kind] = (ct, st_)
                for job in jobs:
                    typ, kind = job[0], job[1]
                    if kind == "q" and not do_q:
                        continue
                    if typ == "rope":
                        _, _, dst, h, c0 = job
                        bx, by = nb(), nb()
                        for c in range(8):
                            P.MM(bx.ap, W[:, c, c0:c0 + 128], hT[:, c, gs], c == 0, c == 7, [W, hT], [bx])
                        for c in range(8):
                            P.MM(by.ap, W[:, c, c0 + 128:c0 + 256], hT[:, c, gs], c == 0, c == 7, [W, hT], [by])
                        ct, st_ = tb[kind]
                        a1, a2 = t1[obi[0] % 2], t2[obi[0] % 2]
                        o = nob()
                        P.TT(a1.ap, bx.ap, ct.ap, ALU.mult, [bx, ct], [a1])
                        P.TT(a2.ap, by.ap, st_.ap, ALU.mult, [by, st_], [a2])
                        P.TT(o.ap, a1.ap, a2.ap, ALU.add, [a1, a2], [o], eng="pool")
                        P.DMA(dst[h:h + 2, :, gs].rearrange("h d t -> (h d) t"), o.ap, reads=[o])
                    elif typ == "plain":
                        _, _, dst, h, c0 = job
                        bx = nb()
                        for c in range(8):
                            P.MM(bx.ap, W[:, c, c0:c0 + 128], hT[:, c, gs], c == 0, c == 7, [W, hT], [bx])
                        o = nob()
                        P.ACT(o.ap, bx.ap, AF.Copy, [bx], [o], scale=(0.125 if kind == "q" else 1.0))
                        P.DMA(dst[h:h + 2, :, gs].rearrange("h d t -> (h d) t"), o.ap, reads=[o])
                    elif typ == "gate":
                        _, _, _, jc, c0 = job
                        bx = nb()
                        for c in range(8):
                            P.MM(bx.ap, W[:, c, c0:c0 + 128], hT[:, c, gs], c == 0, c == 7, [W, hT], [bx])
                        o = nob()
                        P.ACT(o.ap, bx.ap, AF.Sigmoid, [bx], [o])
                        P.DMA(GS[jc * 128:(jc + 1) * 128, gs], o.ap, reads=[o])
                    elif typ == "v":
                        _, _, _, h0, c0, ncol = job
                        nh = ncol // 64
                        for tt in range(4):
                            tok = slice(g * 512 + tt * 128, g * 512 + (tt + 1) * 128)
                            bx = nb()
                            for c in range(8):
                                P.MM(bx[:, 0:ncol], hT[:, c, tok], W[:, c, c0:c0 + ncol], c == 0, c == 7, [W, hT], [bx])
                            o = nob()
                            P.ACT(o[:, 0:ncol], bx[:, 0:ncol], AF.Copy, [bx], [o])
                            P.DMA(VS[h0:h0 + nh, tok, :].rearrange("h t d -> t h d"),
                                  o[:, 0:ncol].rearrange("t (h d) -> t h d", d=64), reads=[o])


    def phase2(l):
        new_phase()
        rows = IN("rows_bf")
        q0 = 0 if l == 0 else HALF
        g0 = q0 // 512
        t0_ = q0 // 128
        Kt = [A.alloc([80, NCTX], BF16) for _ in range(2)]
        Qt = [A.alloc([80, NCTX], BF16) for _ in range(2)]
        Vt = [A.alloc([128, 32, 128], BF16) for _ in range(2)]
        dcp = [A.alloc([128, 512], F32) for _ in range(3)]
        dlo = [A.alloc([64, 512], F32) for _ in range(3)]
        esb = [A.alloc([128, 512], F32) for _ in range(2)]
        spb = [A.alloc([128, 512], BF16) for _ in range(2)]
        argb = [A.alloc([128, 512], F32) for _ in range(2)]
        wsb = [A.alloc([128, 512], BF16) for _ in range(3)]
        Rsb = A.alloc([128, 512], F32)
        rden = A.alloc([64, 512], F32)
        osb = [A.alloc([64, 512], BF16) for _ in range(3)]
        gm = A.alloc([128, 512], F32)
        usb = A.alloc([128, 512], F32)
        top8 = A.alloc([128, 32, 8], F32)
        selpad = A.alloc([128, 32, 80], BF16)
        km = A.alloc([64, 16], F32)
        kmb = A.alloc([64, 16], BF16)
        esink = A.alloc([128, 16], F32)
        cnt = {"w": 0, "o": 0, "z": 0, "r": 0, "bo": 0, "sp": 0, "o2": 0, "dc": 0, "dl": 0}

        def nxt(key, lst):
            t_ = lst[cnt[key] % len(lst)]
            cnt[key] += 1
            return t_

        def load_head(Ksrc, Qsrc, vh, slot, krows):
            K_, Q_, V_ = Kt[slot], Qt[slot], Vt[slot]
            P.DMA(K_[0:64, :], Ksrc, writes=[K_])
            P.DMA(Q_[0:64, q0:NCTX], Qsrc[:, q0:NCTX], writes=[Q_])
            for i in range(4):
                P.DMA(V_[:, 8 * i:8 * i + 8, 0:64], VS[vh, 1024 * i:1024 * (i + 1), :].rearrange("(t p) d -> p t d", p=128),
                      writes=[V_])
            return K_, Q_, V_

        def store_o(bo_, bden_, row0, gs, extra_col=None):
            if bden_ is not None:
                if bden_ == "merged":
                    dc, dl = nxt("dc", dcp), nxt("dl", dlo)
                    P.ACT(dc[64:128, :], bo_[64:128, :], AF.Copy, [bo_], [dc])
                    P.DMA(dl.ap, dc[64:128, :], reads=[dc], writes=[dl])
                    dsrc, dres = dl.ap, dl
                else:
                    dsrc, dres = bden_[0:64, :], bden_
                if extra_col is not None:
                    P.TS(rden.ap, dsrc, extra_col, None, ALU.add, None, [dres, esink], [rden])
                    P.dve(lambda e: e.reciprocal(out=rden.ap, in_=rden.ap), [rden], [rden])
                else:
                    P.dve(lambda e: e.reciprocal(out=rden.ap, in_=dsrc), [dres], [rden])
                o = nxt("o", osb)
                P.TT(o.ap, bo_[0:64, :], rden.ap, ALU.mult, [bo_, rden], [o])
            else:
                o = nxt("o", osb)
                P.ACT(o.ap, bo_[0:64, :], AF.Copy, [bo_], [o])
            P.DMA(OT[row0:row0 + 64, gs], o.ap, reads=[o])

        def run_pipeline(items, nstage):
            n = len(items)
            for i in range(n + nstage - 1):
                for s_ in range(nstage):
                    j = i - s_
                    if 0 <= j < n:
                        items[j][s_]()

        for s_ in range(2):
            P.DMA(Kt[s_][64:80, :], rows[0:16, :], writes=[Kt[s_]])
        P.pool(lambda e: e.memset(selpad.ap, 0.0), [], [selpad])
        for s_ in range(2):
            P.pool(lambda e, s_=s_: e.memset(Vt[s_][:, :, 64:128], 1.0), [], [Vt[s_]])
        nqt = (NCTX - q0) // 128

        def a_load(h):
            load_head(KA[h], QA[h], h, h % 2, 80)

        def a_sel_a(h):
            K_, Q_ = Kt[h % 2], Qt[h % 2]
            P.dve(lambda e: e.tensor_reduce(out=km.ap, in_=K_[0:64, :].rearrange("p (n j) -> p n j", j=256),
                                            axis=AX.X, op=ALU.add), [K_], [km])
            P.ACT(kmb.ap, km.ap, AF.Copy, [km], [kmb])
            bg = bank[7]
            for i in range(nqt):
                ti = t0_ + i
                P.MM(bg[:, 16 * ti:16 * ti + 16], Q_[0:64, ti * 128:(ti + 1) * 128], kmb.ap, True, True, [Q_, kmb], [bg])
            cs = slice(16 * t0_, 512)
            P.TT(gm[:, cs], bg[:, cs], cf[:, F_PB + 16 * t0_:F_PB + 512], ALU.add, [bg, cf], [gm])
            for i in range(nqt):
                ti = t0_ + i
                P.dve(lambda e, ti=ti: e.max(out=top8[:, ti, :], in_=gm[:, 16 * ti:16 * ti + 16]), [gm], [top8])
            for i in range(nqt):
                ti = t0_ + i
                P.STT(usb[:, 16 * ti:16 * ti + 16], gm[:, 16 * ti:16 * ti + 16], top8[:, ti, 2:3],
                      cf[:, F_T1 + 16 * ti:F_T1 + 16 * ti + 16], ALU.is_ge, ALU.mult, [gm, top8, cf], [usb])
            P.TT(selpad[:, t0_:32, 64:80], usb[:, cs].rearrange("p (t n) -> p t n", n=16),
                 cf[:, F_T0 + 16 * t0_:F_T0 + 512].rearrange("p (t n) -> p t n", n=16), ALU.add, [usb, cf], [selpad])

        def a_sel_b(h):
            Q_ = Qt[h % 2]
            for gi in range(nqt // 4):
                bt = bank[5 + (gi % 2)]
                for j in range(4):
                    ti = t0_ + 4 * gi + j
                    P.MM(bt[0:80, 128 * j:128 * j + 128], selpad[:, ti, :], ident, True, True, [selpad, cbf], [bt])
                P.ACT(Q_[64:80, (t0_ + 4 * gi) * 128:(t0_ + 4 * gi + 4) * 128], bt[64:80, :], AF.Copy, [bt], [Q_])

        a_load(0)
        a_sel_a(0)
        a_sel_b(0)
        items = []
        for h in range(4):
            K_, Q_, V_ = Kt[h % 2], Qt[h % 2], Vt[h % 2]
            hp = [(G, J) for G in range(g0, 8) for J in range(4 * G + 4)]
            for pi, (G, J) in enumerate(hp):
                st = {}

                def s0(h=h, pi=pi, G=G, J=J, K_=K_, Q_=Q_, st=st, n=len(hp)):
                    if pi == 4 and h + 1 < 4:
                        a_load(h + 1)
                    if pi == n // 2 and h + 1 < 4:
                        a_sel_a(h + 1)
                    if pi == n - 1 and h + 1 < 4:
                        a_sel_b(h + 1)
                    a = J - 4 * G
                    bs = bank[cnt["z"] % 3]
                    cnt["z"] += 1
                    st["bs"] = bs
                    gs = slice(G * 512, (G + 1) * 512)
                    P.MM(bs.ap, K_[0:80, J * 128:(J + 1) * 128], Q_[0:80, gs], True, a < 0, [K_, Q_], [bs])
                    if a >= 0:
                        P.MM(bs.ap, ident, cbf[:, C_CBA + a * 512:C_CBA + (a + 1) * 512], False, True, [cbf], [bs])

                def s1(st=st):
                    w_ = nxt("w", wsb)
                    st["w"] = w_
                    P.ACT(w_.ap, st["bs"].ap, AF.Exp, [st["bs"]], [w_])

                def s2(h=h, pi=pi, G=G, J=J, V_=V_, st=st, n=len(hp)):
                    nJ = 4 * G + 4
                    bo_ = bank[3 + (G % 2)]
                    w_ = st["w"]
                    P.MM(bo_.ap, V_[:, J, :], w_.ap, J == 0, J == nJ - 1, [V_, w_], [bo_])
                    if J == nJ - 1:
                        store_o(bo_, "merged", h * 64, slice(G * 512, (G + 1) * 512))

                items.append([s0, s1, s2])
        run_pipeline(items, 3)

        P.barrier(bar_scr.ap)
        for s_ in range(2):
            P.DMA(Kt[s_][64:65, :], rows[16:17, :], writes=[Kt[s_]])
            P.DMA(Qt[s_][64:65, :], rows[17:18, :], writes=[Qt[s_]])
        e2b = [A.alloc([128, 2, 512], F32) for _ in range(2)]
        sp2b = [A.alloc([128, 2, 512], BF16) for _ in range(3)]
        ar2b = [A.alloc([128, 2, 512], F32) for _ in range(2)]
        w2b = [A.alloc([128, 2, 512], BF16) for _ in range(3)]

        def c_load(h):
            load_head(KC[h], QC[h], 4 + h, h % 2, 65)

        c_load(0)
        items = []
        cz = [0]
        for h in range(4):
            K_, Q_, V_ = Kt[h % 2], Qt[h % 2], Vt[h % 2]
            hp = [(G, jp) for G in range(g0, 8) for jp in range(2 * G + 2)]
            for pi, (G, jp) in enumerate(hp):
                st = {}
                top = 4 * G + 3
                Ja, Jb = top - 2 * jp, top - 2 * jp - 1

                def s0(h=h, pi=pi, G=G, Ja=Ja, Jb=Jb, K_=K_, Q_=Q_, st=st):
                    if pi == 4 and h + 1 < 4:
                        c_load(h + 1)
                    gs = slice(G * 512, (G + 1) * 512)
                    slot = cz[0] % 2
                    cz[0] += 1
                    bA, bB = bank[2 * slot], bank[2 * slot + 1]
                    st["b"] = (bA, bB, PSB[:, 2 * slot:2 * slot + 2, :])
                    for (b_, J) in ((bA, Ja), (bB, Jb)):
                        a = J - 4 * G
                        P.MM(b_.ap, K_[0:65, J * 128:(J + 1) * 128], Q_[0:65, gs], True, False, [K_, Q_], [b_])
                        if a >= 0:
                            P.MM(b_.ap, ident, cbf[:, C_CBC + a * 512:C_CBC + (a + 1) * 512], False, False, [cbf], [b_])
                    e_ = nxt("r", e2b)
                    sp_ = nxt("sp", sp2b)
                    st["sp"] = sp_
                    P.ACT(e_.ap, st["b"][2], AF.Exp, [bA, bB], [e_])
                    P.ACT(sp_.ap, e_.ap, AF.Ln, [e_, cf], [sp_], bias=cf[:, F_ONE:F_ONE + 1])

                def s1(jp=jp, Jb=Jb, st=st):
                    bA, bB, bAB = st["b"]
                    sp_ = st["sp"]
                    P.MM(bA.ap, ntri, sp_[:, 0, :], False, True, [cbf, sp_], [bA])
                    P.MM(bB.ap, ntri, sp_[:, 1, :], False, False, [cbf, sp_], [bB])
                    P.MM(bB.ap, nones, sp_[:, 0, :], False, True, [cbf, sp_], [bB])
                    if Jb > 0:
                        br = bank[4 + (jp % 2)]
                        P.MM(br.ap, ones_bf, sp_[:, 0, :], True, False, [cbf, sp_], [br])
                        P.MM(br.ap, ones_bf, sp_[:, 1, :], False, True, [cbf, sp_], [br])
                    w_ = nxt("w", w2b)
                    st["w"] = w_
                    if jp == 0:
                        P.ACT(w_.ap, bAB, AF.Exp, [bA, bB], [w_])
                    else:
                        ar = nxt("o2", ar2b)
                        P.TT(ar[:, 0, :], bA.ap, Rsb.ap, ALU.subtract, [bA, Rsb], [ar])
                        P.TT(ar[:, 1, :], bB.ap, Rsb.ap, ALU.subtract, [bB, Rsb], [ar])
                        P.ACT(w_.ap, ar.ap, AF.Exp, [ar], [w_])
                    if Jb > 0:
                        if jp == 0:
                            P.COPY(Rsb.ap, br.ap, [br], [Rsb])
                        else:
                            P.TT(Rsb.ap, br.ap, Rsb.ap, ALU.add, [br, Rsb], [Rsb])

                def s2(h=h, G=G, jp=jp, Ja=Ja, Jb=Jb, V_=V_, st=st):
                    bo_ = bank[6 + (G % 2)]
                    w_ = st["w"]
                    P.MM(bo_[0:64, :], V_[:, Ja, 0:64], w_[:, 0, :], jp == 0, False, [V_, w_], [bo_])
                    P.MM(bo_[0:64, :], V_[:, Jb, 0:64], w_[:, 1, :], False, Jb == 0, [V_, w_], [bo_])
                    if Jb == 0:
                        store_o(bo_, None, 768 + h * 64, slice(G * 512, (G + 1) * 512))

                items.append([s0, s1, s2])
        run_pipeline(items, 3)

        P.barrier(bar_scr.ap)
        P.ACT(esink.ap, cf[:, F_SINK:F_SINK + 16], AF.Exp, [cf], [esink])

        def b_load_kv(kv):
            K_, V_ = Kt[kv], Vt[kv]
            P.DMA(K_[0:64, :], KB[kv], writes=[K_])
            for i in range(4):
                P.DMA(V_[:, 8 * i:8 * i + 8, 0:64], VS[8 + kv, 1024 * i:1024 * (i + 1), :].rearrange("(t p) d -> p t d", p=128),
                      writes=[V_])

        def b_load_q(hq):
            Q_ = Qt[hq % 2]
            P.DMA(Q_[0:64, q0:NCTX], QB[hq][:, q0:NCTX], writes=[Q_])

        b_load_kv(0)
        b_load_kv(1)
        b_load_q(0)
        items = []
        for hq in range(8):
            kv = hq // 4
            K_, V_, Q_ = Kt[kv], Vt[kv], Qt[hq % 2]
            tiles = list(range(4 * g0, 32))
            for pi, I in enumerate(tiles):
                st = {}

                def s0(hq=hq, pi=pi, I=I, K_=K_, Q_=Q_, st=st):
                    if pi == 4 and hq + 1 < 8:
                        b_load_q(hq + 1)
                    qs = slice(I * 128, (I + 1) * 128)
                    bs = bank[cnt["z"] % 3]
                    cnt["z"] += 1
                    st["bs"] = bs
                    if I > 0:
                        P.MM(bs[:, 0:128], K_[0:65, (I - 1) * 128:I * 128], Q_[0:65, qs], True, False, [K_, Q_], [bs])
                        P.MM(bs[:, 0:128], ident, cbf[:, C_CBB + 128:C_CBB + 256], False, True, [cbf], [bs])
                    P.MM(bs[:, 128:256], K_[0:65, qs], Q_[0:65, qs], True, False, [K_, Q_], [bs])
                    P.MM(bs[:, 128:256], ident, cbf[:, C_CBB:C_CBB + 128], False, True, [cbf], [bs])

                def s1(I=I, st=st):
                    w_ = nxt("w", wsb)
                    st["w"] = w_
                    c0 = 0 if I > 0 else 128
                    P.ACT(w_[:, c0:256], st["bs"][:, c0:256], AF.Exp, [st["bs"]], [w_])

                def s2(hq=hq, I=I, V_=V_, st=st):
                    G, j = I // 4, I % 4
                    bo_, bden_ = bank[3 + 2 * (G % 2)], bank[4 + 2 * (G % 2)]
                    w_ = st["w"]
                    oc = slice(128 * j, 128 * j + 128)
                    if I > 0:
                        P.MM(bo_[0:64, oc], V_[:, I - 1, 0:64], w_[:, 0:128], True, False, [V_, w_], [bo_])
                        P.MM(bden_[0:64, oc], ones_bf[:, 0:64], w_[:, 0:128], True, False, [cbf, w_], [bden_])
                    P.MM(bo_[0:64, oc], V_[:, I, 0:64], w_[:, 128:256], I == 0, True, [V_, w_], [bo_])
                    P.MM(bden_[0:64, oc], ones_bf[:, 0:64], w_[:, 128:256], I == 0, True, [cbf, w_], [bden_])
                    if j == 3:
                        store_o(bo_, bden_, 256 + hq * 64, slice(G * 512, (G + 1) * 512),
                                extra_col=esink[0:64, 8 * l + hq:8 * l + hq + 1])

                items.append([s0, s1, s2])
        run_pipeline(items, 3)

    def phase3(l):
        new_phase()
        XTv = XT.rearrange("(c p) t -> p c t", p=128)
        OTv = OT.rearrange("(c p) t -> p c t", p=128)
        GSv = GS.rearrange("(c p) t -> p c t", p=128)
        wproj = A.alloc([128, 8, 1024], BF16)
        wmix = A.alloc([128, 8, 1024], BF16)
        wxq = A.alloc([128, 8, 512], BF16)
        wxo = A.alloc([128, 4, 1024], BF16)
        KmT = A.alloc([128, 4, 256], BF16)
        Vm = A.alloc([128, 2, 512], BF16)
        mark = A.off
        stage = [A.alloc([128, 8, 512], F32) for _ in range(2)]
        wxkv = A.alloc([128, 8, 1024], BF16)
        memx = A.alloc([128, 8, 256], F32)
        memh = A.alloc([128, 8, 256], BF16)
        sq = A.alloc([128, 8, 512], BF16)
        rstd = A.alloc([128, 512], F32)
        si = [0]

        def ld(dram_pcn, dst_ap, dst_t, kc, ncols):
            st_ = stage[si[0] % 2]
            si[0] += 1
            P.DMA(st_[:, 0:kc, 0:ncols], dram_pcn, writes=[st_])
            P.COPY(dst_ap, st_[:, 0:kc, 0:ncols], [st_], [dst_t])

        wp = IN("w_proj")[l].rearrange("(c p) n -> p c n", p=128)
        wm = IN("w_mix_out")[l].rearrange("(c p) n -> p c n", p=128)
        wq = IN("w_xq")[l].rearrange("(c p) n -> p c n", p=128)
        wkv = IN("w_xkv")[l].rearrange("(c p) n -> p c n", p=128)
        wo = IN("w_xo")[l].rearrange("(c p) n -> p c n", p=128)
        for hlf in range(2):
            cs = slice(512 * hlf, 512 * hlf + 512)
            ld(wkv[:, :, cs], wxkv[:, :, cs], wxkv, 8, 512)
        for hlf in range(2):
            cs = slice(512 * hlf, 512 * hlf + 512)
            ld(wp[:, :, cs], wproj[:, :, cs], wproj, 8, 512)
            ld(wm[:, :, cs], wmix[:, :, cs], wmix, 8, 512)
            ld(wo[:, :, cs], wxo[:, :, cs], wxo, 4, 512)
        ld(wq, wxq.ap, wxq, 8, 512)
        P.DMA(memx.ap, IN("memT").rearrange("(c p) t -> p c t", p=128), writes=[memx])
        rmsnorm_group(memx, 256, G_MEM, l, [memh[:, c, :] for c in range(8)], memh, sq, rstd, bank[7])
        for hx in range(4):
            b_ = bank[hx % 2]
            for c in range(8):
                P.MM(b_[:, 0:256], wxkv[:, c, hx * 128:(hx + 1) * 128], memh[:, c, :], c == 0, c == 7, [wxkv, memh], [b_])
            P.ACT(KmT[:, hx, :], b_[:, 0:256], AF.Copy, [b_], [KmT])
        for mt in range(2):
            b_ = bank[2 + mt]
            for c in range(8):
                P.MM(b_.ap, memh[:, c, mt * 128:(mt + 1) * 128], wxkv[:, c, 512:1024], c == 0, c == 7, [wxkv, memh], [b_])
            P.ACT(Vm[:, mt, :], b_.ap, AF.Copy, [b_], [Vm])
        P.barrier(bar_scr.ap)
        A.off = mark
        xg = [A.alloc([128, 8, 512], F32) for _ in range(2)]
        ot = [A.alloc([128, 8, 512], BF16) for _ in range(2)]
        gt = A.alloc([128, 24, 512], BF16)
        mT = A.alloc([128, 8, 512], BF16)
        h2 = A.alloc([128, 8, 512], BF16)
        sq = A.alloc([128, 8, 512], BF16)
        rstd = A.alloc([128, 512], F32)
        qx = A.alloc([128, 4, 512], BF16)
        oc = A.alloc([128, 4, 512], BF16)
        pT = [A.alloc([128, 512], BF16) for _ in range(3)]
        tmpb = [A.alloc([128, 512], BF16) for _ in range(6)]
        rdn = A.alloc([128, 512], F32)
        groups = list(range(8)) if l == 0 else list(range(4, 8))
        xscale = 128 ** -0.5

        def loads(gi):
            g = groups[gi]
            gs = slice(g * 512, (g + 1) * 512)
            P.DMA(xg[gi % 2].ap, XTv[:, :, gs], writes=[xg[gi % 2]])
            P.DMA(ot[gi % 2].ap, OTv[:, :, gs], writes=[ot[gi % 2]])

        loads(0)
        pc = [0]
        for gi, g in enumerate(groups):
            gs = slice(g * 512, (g + 1) * 512)
            X, O_ = xg[gi % 2], ot[gi % 2]
            if gi == 0:
                for part in range(3):
                    P.DMA(gt[:, 8 * part:8 * part + 8, :], GSv[:, 8 * part:8 * part + 8, gs], writes=[gt])
            if gi + 1 < len(groups):
                loads(gi + 1)
            for j in range(8):
                js = slice(j * 128, (j + 1) * 128)
                b0, b1, b2 = bank[0 + 3 * (j % 2)], bank[1 + 3 * (j % 2)], bank[2 + 3 * (j % 2)]
                for c in range(0, 2):
                    P.MM(b0.ap, wproj[:, c, js], O_[:, c, :], c == 0, c == 1, [wproj, O_], [b0])
                for c in range(2, 6):
                    P.MM(b1.ap, wproj[:, c, js], O_[:, c, :], c == 2, c == 5, [wproj, O_], [b1])
                for c in range(6, 8):
                    P.MM(b2.ap, wproj[:, c, js], O_[:, c, :], c == 6, c == 7, [wproj, O_], [b2])
                ta, tb_, tc = tmpb[(3 * j) % 6], tmpb[(3 * j + 1) % 6], tmpb[(3 * j + 2) % 6]
                P.TT(ta.ap, b0.ap, gt[:, j, :], ALU.mult, [b0, gt], [ta])
                P.TT(tb_.ap, b1.ap, gt[:, 8 + j, :], ALU.mult, [b1, gt], [tb_])
                P.TT(tc.ap, b2.ap, gt[:, 16 + j, :], ALU.mult, [b2, gt], [tc])
                P.TT(ta.ap, ta.ap, tb_.ap, ALU.add, [ta, tb_], [ta])
                P.TT(mT[:, j, :], ta.ap, tc.ap, ALU.add, [ta, tc], [mT])
            if gi + 1 < len(groups):
                gn = groups[gi + 1]
                for part in range(3):
                    P.DMA(gt[:, 8 * part:8 * part + 8, :], GSv[:, 8 * part:8 * part + 8, gn * 512:(gn + 1) * 512], writes=[gt])
            for j in range(8):
                js = slice(j * 128, (j + 1) * 128)
                b_ = bank[j % 4]
                for c in range(8):
                    P.MM(b_.ap, wmix[:, c, js], mT[:, c, :], c == 0, c == 7, [wmix, mT], [b_])
                P.TT(X[:, j, :], b_.ap, X[:, j, :], ALU.add, [b_, X], [X])
            rmsnorm_group(X, 512, G_CROSS, l, [h2[:, c, :] for c in range(8)], h2, sq, rstd, bank[7])
            for hx in range(4):
                b_ = bank[hx % 4]
                for c in range(8):
                    P.MM(b_.ap, wxq[:, c, hx * 128:(hx + 1) * 128], h2[:, c, :], c == 0, c == 7, [wxq, h2], [b_])
                P.ACT(qx[:, hx, :], b_.ap, AF.Copy, [b_], [qx])
            for hx in range(4):
                bo_, bd_ = bank[4 + 2 * (hx % 2)], bank[5 + 2 * (hx % 2)]
                for mt in range(2):
                    bs = bank[(2 * hx + mt) % 4]
                    P.MM(bs.ap, KmT[:, hx, mt * 128:(mt + 1) * 128], qx[:, hx, :], True, True, [KmT, qx], [bs])
                    p_ = pT[pc[0] % 3]
                    pc[0] += 1
                    P.ACT(p_.ap, bs.ap, AF.Exp, [bs], [p_], scale=xscale)
                    P.MM(bo_.ap, Vm[:, mt, hx * 128:(hx + 1) * 128], p_.ap, mt == 0, mt == 1, [Vm, p_], [bo_])
                    P.MM(bd_.ap, ones_bf, p_.ap, mt == 0, mt == 1, [cbf, p_], [bd_])
                P.dve(lambda e, bd_=bd_: e.reciprocal(out=rdn.ap, in_=bd_.ap), [bd_], [rdn])
                P.TT(oc[:, hx, :], bo_.ap, rdn.ap, ALU.mult, [bo_, rdn], [oc])
            for j in range(8):
                js = slice(j * 128, (j + 1) * 128)
                b_ = bank[j % 4]
                for hx in range(4):
                    P.MM(b_.ap, wxo[:, hx, js], oc[:, hx, :], hx == 0, hx == 3, [wxo, oc], [b_])
                P.TT(X[:, j, :], b_.ap, X[:, j, :], ALU.add, [b_, X], [X])
            P.DMA(XTv[:, :, gs], X.ap, reads=[X])

    def phase4(l, last):
        new_phase()
        XTv = XT.rearrange("(c p) t -> p c t", p=128)
        moe = (l % 2 == 1)
        li = l // 2
        FB = 256
        NFC = FB // 128
        if moe:
            F_, nexp = D_FFE, NE
            wgs = [IN("moe_gate")[li, e_].rearrange("(c p) n -> p c n", p=128) for e_ in range(NE)]
            wus = [IN("moe_up")[li, e_].rearrange("(c p) n -> p c n", p=128) for e_ in range(NE)]
            wds = [IN("moe_down")[li, e_].rearrange("(c p) n -> p c n", p=128) for e_ in range(NE)]
        else:
            F_, nexp = D_FF, 1
            wgs = [IN("ffn_gate")[li].rearrange("(c p) n -> p c n", p=128)]
            wus = [IN("ffn_up")[li].rearrange("(c p) n -> p c n", p=128)]
            wds = [IN("ffn_down")[li].rearrange("(c p) n -> p c n", p=128)]
        nfb = F_ // FB
        yacc = A.alloc([128, 8, 2048], F32)
        h3 = A.alloc([128, 8, 2048], BF16)
        stage = [A.alloc([128, 8, FB], F32) for _ in range(2)]
        wg = [A.alloc([128, 8, FB], BF16) for _ in range(2)]
        wu = [A.alloc([128, 8, FB], BF16) for _ in range(2)]
        wd = [A.alloc([128, NFC, 1024], BF16) for _ in range(2)]
        aT = [A.alloc([128, NFC, 512], BF16) for _ in range(3)]
        sgs = [A.alloc([128, 512], BF16) for _ in range(2)]
        cbs = [A.alloc([128, 512], BF16) for _ in range(4)]
        sq = A.alloc([128, 8, 512], BF16)
        rstd = A.alloc([128, 512], F32)
        hf = A.alloc([128, 8, 512], F32)
        wr = A.alloc([128, 8, 8], F32)
        lg = A.alloc([128, 16, 8], F32)
        top8 = A.alloc([128, 16, 8], F32)
        rt = [A.alloc([128, 16], F32) for _ in range(3)]
        comb = A.alloc([128, 16, 8], F32)
        ctmp = A.alloc([128, 8], F32)
        dg = [A.alloc([128, 128], F32) for _ in range(2)]
        ident32 = cf[:, F_ID:F_ID + 128]
        ones32 = cf[:, F_ONES32:F_ONES32 + 128]
        sgroups = [0, 1] if l == 0 else [1]
        if moe:
            P.DMA(wr.ap, IN("moe_router")[li].rearrange("(c p) n -> p c n", p=128), writes=[wr])
        for sgi in sgroups:
            t0_ = sgi * 2048
            for g in range(4):
                P.DMA(yacc[:, :, g * 512:(g + 1) * 512], XTv[:, :, t0_ + g * 512:t0_ + (g + 1) * 512], writes=[yacc])
            for g in range(4):
                gl = slice(g * 512, (g + 1) * 512)
                xv = T(yacc.ap[:, :, gl], res=yacc.res)
                if moe:
                    rmsnorm_group(xv, 512, G_FFN, l, [hf[:, c, :] for c in range(8)], hf, sq, rstd, bank[7])
                    for c in range(8):
                        P.ACT(h3[:, c, gl], hf[:, c, :], AF.Copy, [hf], [h3])
                    for tt in range(4):
                        b_ = bank[6]
                        ti = 4 * g + tt
                        for c in range(8):
                            P.MM(b_[:, 8 * tt:8 * tt + 8], hf[:, c, tt * 128:(tt + 1) * 128], wr[:, c, :], c == 0, c == 7,
                                 [hf, wr], [b_])
                    P.COPY(lg[:, 4 * g:4 * g + 4, :], bank[6][:, 0:32].rearrange("p (t e) -> p t e", e=8), [bank[6]], [lg])
                else:
                    rmsnorm_group(xv, 512, G_FFN, l, [h3[:, c, gl] for c in range(8)], h3, sq, rstd, bank[7])
            if moe:
                for ti in range(16):
                    P.dve(lambda e, ti=ti: e.max(out=top8[:, ti, :], in_=lg[:, ti, :]), [lg], [top8])
                P.TT(rt[0].ap, top8[:, :, 1], top8[:, :, 0], ALU.subtract, [top8], [rt[0]])
                P.ACT(rt[0].ap, rt[0].ap, AF.Exp, [rt[0]], [rt[0]])
                P.TS(rt[1].ap, rt[0].ap, 1.0, None, ALU.add, None, [rt[0]], [rt[1]])
                P.dve(lambda e: e.reciprocal(out=rt[1].ap, in_=rt[1].ap), [rt[1]], [rt[1]])
                P.TT(rt[2].ap, rt[0].ap, rt[1].ap, ALU.mult, [rt[0], rt[1]], [rt[2]])
                for ti in range(16):
                    P.TS(comb[:, ti, :], lg[:, ti, :], top8[:, ti, 0:1], rt[1][:, ti:ti + 1], ALU.is_equal, ALU.mult,
                         [lg, top8, rt[1]], [comb])
                    P.TS(ctmp.ap, lg[:, ti, :], top8[:, ti, 1:2], rt[2][:, ti:ti + 1], ALU.is_equal, ALU.mult,
                         [lg, top8, rt[2]], [ctmp])
                    P.TT(comb[:, ti, :], comb[:, ti, :], ctmp.ap, ALU.add, [comb, ctmp], [comb])
            steps = [(e_, fb) for e_ in range(nexp) for fb in range(nfb)]
            sti = [0]

            def issue(k, part):
                e_, fb = steps[k]
                fs = slice(fb * FB, (fb + 1) * FB)
                (src, dst, kc, nco) = ((wgs[e_][:, :, fs], wg[k % 2], 8, FB), (wus[e_][:, :, fs], wu[k % 2], 8, FB),
                                       (wds[e_][:, NFC * fb:NFC * fb + NFC, :], wd[k % 2], NFC, 1024))[part]
                st_ = stage[sti[0] % 2]
                sti[0] += 1
                stv = T(st_.ap.rearrange("p a b -> p (a b)").rearrange("p (a b) -> p a b", b=nco), res=st_.res)
                P.DMA(stv[:, 0:kc, :], src, writes=[st_])
                P.ACT(dst.ap, stv[:, 0:kc, :], AF.Copy, [st_], [dst])

            for part_ in range(3):
                issue(0, part_)
            ai = [0]
            bdi = [0]
            bdbanks = [bank[4], bank[5], bank[6], bank[7]]
            items = []
            for k, (e_, fb) in enumerate(steps):
                for g in range(4):
                    st = {}

                    def gu(fc, k, g, st):
                        G_, U_ = wg[k % 2], wu[k % 2]
                        gl = slice(g * 512, (g + 1) * 512)
                        a_ = st["a"]
                        bg_, bu_ = bank[2 * (fc % 2)], bank[2 * (fc % 2) + 1]
                        for c in range(8):
                            P.MM(bg_.ap, G_[:, c, fc * 128:(fc + 1) * 128], h3[:, c, gl], c == 0, c == 7, [G_, h3], [bg_])
                        for c in range(8):
                            P.MM(bu_.ap, U_[:, c, fc * 128:(fc + 1) * 128], h3[:, c, gl], c == 0, c == 7, [U_, h3], [bu_])
                        s_ = sgs[fc % 2]
                        P.ACT(s_.ap, bg_.ap, AF.Silu, [bg_], [s_])
                        if moe:
                            P.TT(s_.ap, s_.ap, cbs[g].ap, ALU.mult, [s_, cbs[g]], [s_])
                        P.TT(a_[:, fc, :], bu_.ap, s_.ap, ALU.mult, [bu_, s_], [a_])

                    def s0a(k=k, e_=e_, fb=fb, g=g, st=st):
                        if g >= 1 and k + 1 < len(steps):
                            issue(k + 1, g - 1)
                        if moe and fb == 0:
                            cb_ = cbs[g]
                            for tt in range(4):
                                d_ = dg[tt % 2]
                                P.TS(d_.ap, ident32, comb[:, 4 * g + tt, e_:e_ + 1], None, ALU.mult, None, [cf, comb], [d_])
                                P.MM(bank[6][:, tt * 128:(tt + 1) * 128], ones32, d_.ap, True, True, [cf, d_], [bank[6]])
                            P.ACT(cb_.ap, bank[6].ap, AF.Copy, [bank[6]], [cb_])
                        st["a"] = aT[ai[0] % 3]
                        ai[0] += 1
                        gu(0, k, g, st)

                    def s0b(k=k, g=g, st=st):
                        for fc in range(1, NFC):
                            gu(fc, k, g, st)

                    def down(j0, j1, k, g, st):
                        D_ = wd[k % 2]
                        a_ = st["a"]
                        gl = slice(g * 512, (g + 1) * 512)
                        for j in range(j0, j1):
                            bd_ = bdbanks[bdi[0] % 4]
                            bdi[0] += 1
                            for fc in range(NFC):
                                P.MM(bd_.ap, D_[:, fc, j * 128:(j + 1) * 128], a_[:, fc, :], fc == 0, fc == NFC - 1, [D_, a_], [bd_])
                            P.TT(yacc[:, j, gl], bd_.ap, yacc[:, j, gl], ALU.add, [bd_, yacc], [yacc])

                    def s1a(k=k, g=g, st=st):
                        down(0, 4, k, g, st)

                    def s1b(k=k, g=g, st=st):
                        down(4, 8, k, g, st)

                    items.append([s0a, s0b, s1a, s1b])
            n_it = len(items)
            for i in range(n_it + 1):
                if i < n_it:
                    items[i][0]()
                if i >= 1:
                    items[i - 1][2]()
                if i < n_it:
                    items[i][1]()
                if i >= 1:
                    items[i - 1][3]()
            if not last:
                for g in range(4):
                    P.DMA(XTv[:, :, t0_ + g * 512:t0_ + (g + 1) * 512], yacc[:, :, g * 512:(g + 1) * 512], reads=[yacc])
            else:
                oT = out_T.rearrange("(c p) t -> p c t", p=128)
                for g in range(4):
                    gl = slice(g * 512, (g + 1) * 512)
                    xv = T(yacc.ap[:, :, gl], res=yacc.res)
                    rmsnorm_group(xv, 512, G_FINAL, 0, [hf[:, c, :] for c in range(8)], hf, sq, rstd, bank[7])
                    P.DMA(oT[:, :, gl], hf.ap, reads=[hf])

    cbs_cur = [None] * 4

    phases = []
    for l in range(n_layers):
        phases.append(("p1_%d" % l, lambda l=l: phase1(l)))
        phases.append(("p2_%d" % l, lambda l=l: phase2(l)))
        phases.append(("p3_%d" % l, lambda l=l: phase3(l)))
        phases.append(("p4_%d" % l, lambda l=l: phase4(l, l == n_layers - 1 and n_layers == 2)))
    for name, fn in phases:
        fn()
        if stop_phase is not None and stop_phase.replace("p1a", "p1") == name:
            break
    if dbg:
        new_phase()
        srcs = {"QA": QA, "KA": KA, "QB": QB, "KB": KB, "QC": QC, "KC": KC, "VS": VS, "GS": GS, "OT": OT, "XT": XT}
        for name, ap in dbg.items():
            if name.replace("dbg_", "") not in srcs:
                continue
            s = srcs[name.replace("dbg_", "")]
            P.DMA(ap, s)
    P.finalize()
    P.used_inputs = list(used_inputs.keys())
    return nc, P


def _tables(role):
    bf = ml_dtypes.bfloat16
    cb = np.zeros((128, NCBF), np.float32)
    cb[:, C_ID:C_ID + 128] = np.eye(128)
    cb[:, C_ONES:C_ONES + 128] = 1.0
    j = np.arange(128)[:, None]
    s_ = np.arange(128)[None, :]
    cb[:, C_NTRI:C_NTRI + 128] = np.where(j >= s_, -1.0, 0.0)
    cb[:, C_NONES:C_NONES + 128] = -1.0
    k = np.arange(128)[:, None]
    q = np.arange(128)[None, :]
    for a in range(4):
        for c in range(4):
            strict = np.where((a > c) | ((a == c) & (k >= q)), NEG, 0.0)
            nonstrict = np.where((a > c) | ((a == c) & (k > q)), NEG, 0.0)
            cb[:, C_CBC + a * 512 + c * 128:C_CBC + a * 512 + (c + 1) * 128] = strict
            cb[:, C_CBA + a * 512 + c * 128:C_CBA + a * 512 + (c + 1) * 128] = nonstrict
    cb[:, C_CBB:C_CBB + 128] = np.where(k > q, NEG, 0.0)
    cb[:, C_CBB + 128:C_CBB + 256] = np.where(k <= q, NEG, 0.0)
    cbf = cb.astype(bf)
    pbt = np.zeros((16, 16), np.float32)
    t1 = np.zeros((16, 16), np.float32)
    t0 = np.zeros((16, 16), np.float32)
    for own in range(16):
        for n in range(16):
            if role == 1 or own < 8:
                valid = n < own
            else:
                valid = 8 <= n < own
            pbt[own, n] = 0.0 if valid else -1e30
            t1[own, n] = -NEG if valid else 0.0
            t0[own, n] = NEG if valid else (0.0 if n == own else NEG)
    rows = np.zeros((18, NCTX), np.float32)
    pos = np.arange(NCTX)
    rows[np.arange(NCTX) // 256, np.arange(NCTX)] = 1.0
    if role == 0:
        rows[16, :HALF] = NEG
        rows[17, HALF:] = 1.0
        pos = np.where(pos >= HALF, pos - HALF, pos)
    else:
        rows[17, :] = 1.0
    rows_bf = rows.astype(bf)
    half = 32
    inv_freq = (np.float32(10000.0) ** (-np.arange(half, dtype=np.float32) / np.float32(half))).astype(np.float32)
    ang = pos.astype(np.float32)[:, None] * inv_freq[None, :]
    cos = np.cos(ang).astype(np.float32).T
    sin = np.sin(ang).astype(np.float32).T
    cosT = np.concatenate([cos, cos], 0)
    sinT = np.concatenate([-sin, sin], 0)
    rope = np.stack([cosT * np.float32(0.125), sinT * np.float32(0.125), cosT, sinT]).astype(np.float32)
    return cbf, pbt, t1, t0, rows_bf, rope


def _prep_inputs(inp):
    f = lambda a: np.ascontiguousarray(np.asarray(a, dtype=np.float32))
    x = f(inp["x"])
    mem = f(inp["mem"])
    cols = _w_in_cols()
    w_in = f(inp["w_in"])
    w_in_ext = np.zeros((2, D, NCOLX), np.float32)
    valid = cols >= 0
    w_in_ext[:, :, valid] = w_in[:, :, cols[valid]]
    w_proj = np.concatenate([f(inp["w_proj_a"]), f(inp["w_proj_b"]), f(inp["w_proj_c"])], axis=1)
    gains = np.zeros((128, 72), np.float32)
    for off, key in ((G_MIX, "norm_mix"), (G_CROSS, "norm_cross"), (G_MEM, "norm_mem"), (G_FFN, "norm_ffn")):
        g = f(inp[key])
        for l in range(2):
            gains[:, off + 8 * l:off + 8 * l + 8] = g[l].reshape(8, 128).T
    gains[:, G_FINAL:G_FINAL + 8] = f(inp["final_norm"]).reshape(8, 128).T
    sinks = f(inp["sinks"]).reshape(1, 16)
    shared = {
        "w_in_ext": w_in_ext, "w_proj": w_proj, "w_mix_out": f(inp["w_mix_out"]),
        "w_xq": f(inp["w_xq"]), "w_xkv": f(inp["w_xkv"]), "w_xo": f(inp["w_xo"]),
        "ffn_gate": f(inp["ffn_gate"]), "ffn_up": f(inp["ffn_up"]), "ffn_down": f(inp["ffn_down"]),
        "moe_router": f(inp["moe_router"]), "moe_gate": f(inp["moe_gate"]), "moe_up": f(inp["moe_up"]),
        "moe_down": f(inp["moe_down"]),
    }
    in_maps = []
    tabs = [_tables(0), _tables(1)]
    for c in range(8):
        b, role = c // 2, c % 2
        cbf, pbt, t1, t0, rows_bf, rope = tabs[role]
        if role == 1:
            ctx = x[b]
        else:
            ctx = np.concatenate([np.zeros((HALF, D), np.float32), x[b, :HALF]], axis=0)
        cf = np.zeros((128, NCF), np.float32)
        cf[:, F_ID:F_ID + 128] = np.eye(128, dtype=np.float32)
        own_of_tile = np.arange(32) // 2
        cf[:, F_PB:F_PB + 512] = pbt[own_of_tile].reshape(1, 512)
        cf[:, F_T1:F_T1 + 512] = t1[own_of_tile].reshape(1, 512)
        cf[:, F_T0:F_T0 + 512] = t0[own_of_tile].reshape(1, 512)
        cf[:, F_ONE] = 1.0
        cf[:, F_ONES32:F_ONES32 + 128] = 1.0
        cf[:, F_GAIN:F_GAIN + 72] = gains
        cf[:, F_SINK:F_SINK + 16] = sinks
        cf[:, F_EPS] = EPS
        m = dict(shared)
        m.update({"xT": np.ascontiguousarray(ctx.T), "memT": np.ascontiguousarray(mem[b].T),
                  "cbf": cbf, "cf32": cf, "rows_bf": rows_bf, "rope": rope})
        in_maps.append(m)
    return in_maps


_NC_CACHE = {}


def kernel(**inputs):
    in_maps = _prep_inputs(inputs)
    if "nc" not in _NC_CACHE:
        _NC_CACHE["nc"] = build()[0]
    nc = _NC_CACHE["nc"]
    res = run_bass_kernel_spmd(nc, in_maps, core_ids=list(range(8)))
    out = np.zeros((4, SEQ, D), np.float32)
    for c in range(8):
        b, role = c // 2, c % 2
        o = np.asarray(res.results[c]["outT"]).T
        if role == 0:
            out[b, :HALF] = o
        else:
            out[b, HALF:] = o
    return out
```

```python
import contextlib
import numpy as np
import ml_dtypes
import concourse.bass as bass
import concourse.mybir as mybir
from concourse.bass_utils import run_bass_kernel_spmd

F32 = mybir.dt.float32
BF16 = mybir.dt.bfloat16
U8 = mybir.dt.uint8
AF = mybir.ActivationFunctionType
ALU = mybir.AluOpType
AX = mybir.AxisListType

ENGS = ("pe", "act", "dve", "pool", "sp")
N_DMA_SEMS = 56
SAME_SYNC = {"pe": False, "act": True, "dve": True, "pool": True, "sp": False}


class Res:
    __slots__ = ("name", "w", "r", "excl")

    def __init__(self, name="", excl=False):
        self.name = name
        self.w = None
        self.r = []
        self.excl = excl


class Op:
    __slots__ = ("eng", "fn", "reads", "writes", "dma", "idx", "deps", "signal",
                 "cnt", "dsem", "dcnt", "waits")

    def __init__(self, eng, fn, reads, writes, dma):
        self.eng = eng
        self.fn = fn
        self.reads = reads
        self.writes = writes
        self.dma = dma
        self.deps = []
        self.signal = False
        self.cnt = 0
        self.dsem = -1
        self.dcnt = 0
        self.waits = []


class T:
    __slots__ = ("ap", "res")

    def __init__(self, ap, res=None, excl=False):
        self.ap = ap
        self.res = res if res is not None else Res(excl=excl)

    def __getitem__(self, k):
        return self.ap[k]


class Prog:
    def __init__(self, nc):
        self.nc = nc
        self.ops = []
        self.stack = contextlib.ExitStack()
        self.gall = Res("ALL")

    def add(self, eng, fn, reads=(), writes=(), dma=False):
        rr = [x.res if isinstance(x, T) else x for x in reads]
        ww = [x.res if isinstance(x, T) else x for x in writes]
        rr.append(self.gall)
        op = Op(eng, fn, tuple(rr), tuple(ww), dma)
        op.idx = len(self.ops)
        self.ops.append(op)
        return op

    def pe(self, fn, reads=(), writes=()):
        return self.add("pe", fn, reads, writes)

    def act(self, fn, reads=(), writes=()):
        return self.add("act", fn, reads, writes)

    def dve(self, fn, reads=(), writes=()):
        return self.add("dve", fn, reads, writes)

    def pool(self, fn, reads=(), writes=()):
        return self.add("pool", fn, reads, writes)

    def dma(self, fn, reads=(), writes=(), q="sp"):
        return self.add(q, fn, reads, writes, dma=True)

    def barrier(self, scratch_ap):
        op = Op("pool", lambda e: e.memset(scratch_ap, 0.0), (), (self.gall,), False)
        op.idx = len(self.ops)
        self.ops.append(op)


    def MM(self, out, lhsT, rhs, start, stop, reads, writes):
        return self.pe(lambda e: e.matmul(out, lhsT=lhsT, rhs=rhs, start=start, stop=stop), reads, writes)

    def ACT(self, out, in_, func, reads, writes, scale=None, bias=None, eng="act"):
        kw = {}
        if scale is not None:
            kw["scale"] = scale
        if bias is not None:
            kw["bias"] = bias
        return self.add(eng, lambda e: e.activation(out=out, in_=in_, func=func, **kw), reads, writes)

    def TT(self, out, in0, in1, op, reads, writes, eng="dve"):
        return self.add(eng, lambda e: e.tensor_tensor(out=out, in0=in0, in1=in1, op=op), reads, writes)

    def TS(self, out, in0, s1, s2, op0, op1, reads, writes, eng="dve"):
        if op1 is None:
            return self.add(eng, lambda e: e.tensor_scalar(out=out, in0=in0, scalar1=s1, scalar2=None, op0=op0), reads, writes)
        return self.add(eng, lambda e: e.tensor_scalar(out=out, in0=in0, scalar1=s1, scalar2=s2, op0=op0, op1=op1), reads, writes)

    def STT(self, out, in0, scalar, in1, op0, op1, reads, writes):
        return self.dve(lambda e: e.scalar_tensor_tensor(out=out, in0=in0, scalar=scalar, in1=in1, op0=op0, op1=op1), reads, writes)

    def COPY(self, out, in_, reads, writes, eng="dve"):
        return self.add(eng, lambda e: e.tensor_copy(out=out, in_=in_), reads, writes)

    def DMA(self, out, in_, reads=(), writes=(), q="sp"):
        return self.dma(lambda e: e.dma_start(out=out, in_=in_), reads, writes, q=q)

    def finalize(self):
        nc = self.nc
        ops = self.ops
        last_op = {}
        for op in ops:
            deps = {}
            for r in op.reads:
                if r.w is not None:
                    deps[r.w.idx] = r.w
                if r.excl:
                    for rd in r.r:
                        if rd.eng != op.eng:
                            deps[rd.idx] = rd
            for w in op.writes:
                if w.w is not None:
                    deps[w.w.idx] = w.w
                if w is self.gall:
                    for rd in w.r:
                        if rd.dma:
                            deps[rd.idx] = rd
                    for lo in last_op.values():
                        deps[lo.idx] = lo
                else:
                    for rd in w.r:
                        deps[rd.idx] = rd
            if not op.dma:
                last_op[op.eng] = op
            for r in op.reads:
                if op.dma or r is self.gall:
                    r.r.append(op)
                else:
                    for i_, rd in enumerate(r.r):
                        if (not rd.dma) and rd.eng == op.eng:
                            r.r[i_] = op
                            break
                    else:
                        r.r.append(op)
            for w in op.writes:
                w.w = op
                w.r = []
            deps.pop(op.idx, None)
            op.deps = list(deps.values())
        ndma = 0
        for op in ops:
            if op.dma:
                op.dsem = ndma % N_DMA_SEMS
                op.dcnt = 16 * (ndma // N_DMA_SEMS + 1)
                ndma += 1

        def skip(d, op):
            return (not d.dma) and (not op.dma) and d.eng == op.eng and not SAME_SYNC[d.eng]

        for op in ops:
            for d in op.deps:
                if d.dma or skip(d, op):
                    continue
                d.signal = True
        cnt = {e: 0 for e in ENGS}
        for op in ops:
            if op.dma:
                continue
            if op.signal:
                cnt[op.eng] += 1
            op.cnt = cnt[op.eng]
        waited = {e: {} for e in ENGS}
        for op in ops:
            need = {}
            for d in op.deps:
                if d.dma:
                    key = ("d", d.dsem)
                    val = d.dcnt
                else:
                    if skip(d, op):
                        continue
                    key = ("e", d.eng)
                    val = d.cnt
                if need.get(key, 0) < val:
                    need[key] = val
            if op.dma and op.dcnt > 16:
                key = ("d", op.dsem)
                val = op.dcnt - 16
                if need.get(key, 0) < val:
                    need[key] = val
            wl = []
            wd = waited[op.eng]
            for key, val in need.items():
                if wd.get(key, 0) >= val:
                    continue
                wd[key] = val
                wl.append((key, val))
            op.waits = wl
        st = self.stack
        esem = {e: st.enter_context(nc.semaphore("sem_" + e)) for e in ENGS}
        dsem = [st.enter_context(nc.semaphore("dsem%d" % i)) for i in range(min(N_DMA_SEMS, max(ndma, 1)))]
        by_eng = {e: [o for o in ops if o.eng == e] for e in ENGS}
        self.stats = {e: len(by_eng[e]) for e in ENGS}
        self.stats["waits"] = sum(len(o.waits) for o in ops)
        self.stats["signals"] = sum(1 for o in ops if o.signal)
        self.stats["ndma"] = ndma

        def emit(e, lst):
            for op in lst:
                for key, val in op.waits:
                    s = dsem[key[1]] if key[0] == "d" else esem[key[1]]
                    e.wait_ge(s, val)
                inst = op.fn(e)
                if op.dma:
                    inst.then_inc(dsem[op.dsem], 16)
                elif op.signal:
                    inst.then_inc(esem[op.eng], 1)

        block = st.enter_context(nc.Block())

        @block.tensor
        def _(e):
            emit(e, by_eng["pe"])

        @block.scalar
        def _(e):
            emit(e, by_eng["act"])

        @block.vector
        def _(e):
            emit(e, by_eng["dve"])

        @block.gpsimd
        def _(e):
            emit(e, by_eng["pool"])

        @block.sync
        def _(e):
            emit(e, by_eng["sp"])
            last = {}
            for op in ops:
                if op.dma:
                    last[op.dsem] = op.dcnt
            wd = waited["sp"]
            for s, v in last.items():
                if wd.get(("d", s), 0) < v:
                    e.wait_ge(dsem[s], v)

        st.close()


class Arena:
    def __init__(self, SB, nbytes):
        self.SB = SB
        self.nbytes = nbytes
        self.off = 0

    def alloc(self, shape, dtype, parts=None):
        esz = 2 if dtype == BF16 else 4
        parts = shape[0]
        free = list(shape[1:])
        n = 1
        for s in free:
            n *= s
        nb = (n * esz + 31) // 32 * 32
        assert self.off + nb <= self.nbytes, ("SBUF overflow", self.off, nb, self.nbytes)
        ap = self.SB[:, self.off:self.off + n * esz].bitcast(dtype)
        self.off += nb
        if len(free) == 2:
            ap = ap.rearrange("p (a b) -> p a b", b=free[1])
        elif len(free) == 3:
            ap = ap.rearrange("p (a b c) -> p a b c", b=free[1], c=free[2])
        if parts != 128:
            ap = ap[0:parts]
        return T(ap)


D = 1024
SEQ = 4096
NCTX = 4096
HALF = 2048
HD = 64
NEG = -30000.0
EPS = 1e-6
D_FF = 2816
D_FFE = 3584
NE = 8
SB_BYTES = 212480

NBLK = 13
NCOLX = NBLK * 512
C_ID, C_ONES, C_NTRI, C_NONES = 0, 128, 256, 384
C_CBC, C_CBA, C_CBB = 512, 512 + 2048, 512 + 4096
NCBF = 512 + 4096 + 256
F_ID, F_PB, F_T1, F_T0, F_GAIN, F_SINK, F_EPS, F_ONE, F_ONES32 = 0, 128, 640, 1152, 1664, 1736, 1752, 1753, 1760
NCF = 1888
G_MIX, G_CROSS, G_MEM, G_FFN, G_FINAL = 0, 16, 32, 48, 64


def _w_in_cols():
    qa, ka, va, qb, kb, vb, qc, kc, vc, gt = 0, 256, 512, 768, 1280, 1408, 1536, 1792, 2048, 2304
    sw = [(j + 32) % 64 for j in range(64)]
    cols = []

    def head(base, h, swapped=False):
        if swapped:
            return [base + 64 * h + j for j in sw]
        return [base + 64 * h + j for j in range(64)]

    def pair(base, h0):
        return head(base, h0) + head(base, h0 + 1) + head(base, h0, True) + head(base, h0 + 1, True)

    cols += pair(qa, 0) + pair(qa, 2)
    cols += pair(ka, 0) + pair(ka, 2)
    cols += pair(qb, 0) + pair(qb, 2)
    cols += pair(qb, 4) + pair(qb, 6)
    cols += pair(kb, 0)
    for h in range(4):
        cols += head(qc, h)
    for h in range(4):
        cols += head(kc, h)
    cols += list(range(va, va + 256))
    cols += list(range(vc, vc + 256))
    cols += list(range(vb, vb + 128))
    cols += [-1] * 128
    cols += list(range(gt, gt + 3072))
    assert len(cols) == NCOLX
    return np.array(cols)


def build(n_layers=2, stop_phase=None, debug=()):
    nc = bass.Bass("TRN2", target_bir_lowering=False)

    def din(name, shape, dt=F32):
        return nc.dram_tensor(name, list(shape), dt, kind="ExternalInput").ap()

    def dscr(name, shape, dt):
        return nc.dram_tensor(name, list(shape), dt, kind="Internal").ap()

    in_shapes = {
        "xT": ([D, NCTX], F32), "memT": ([D, 256], F32), "w_in_ext": ([2, D, NCOLX], F32),
        "w_proj": ([2, D, D], F32), "w_mix_out": ([2, D, D], F32), "w_xq": ([2, D, 512], F32),
        "w_xkv": ([2, D, 1024], F32), "w_xo": ([2, 512, D], F32),
        "ffn_gate": ([1, D, D_FF], F32), "ffn_up": ([1, D, D_FF], F32), "ffn_down": ([1, D_FF, D], F32),
        "moe_router": ([1, D, NE], F32), "moe_gate": ([1, NE, D, D_FFE], F32),
        "moe_up": ([1, NE, D, D_FFE], F32), "moe_down": ([1, NE, D_FFE, D], F32),
        "cbf": ([128, NCBF], BF16), "cf32": ([128, NCF], F32), "rows_bf": ([18, NCTX], BF16),
        "rope": ([4, HD, NCTX], F32),
    }
    used_inputs = {}

    def IN(name):
        if name not in used_inputs:
            shp, dt = in_shapes[name]
            used_inputs[name] = din(name, shp, dt)
        return used_inputs[name]

    out_T = nc.dram_tensor("outT", [D, HALF], F32, kind="ExternalOutput").ap()

    XT = dscr("XT", [D, NCTX], F32)
    QA = dscr("QA", [4, HD, NCTX], BF16)
    KA = dscr("KA", [4, HD, NCTX], BF16)
    QB = dscr("QB", [8, HD, NCTX], BF16)
    KB = dscr("KB", [2, HD, NCTX], BF16)
    QC = dscr("QC", [4, HD, NCTX], BF16)
    KC = dscr("KC", [4, HD, NCTX], BF16)
    VS = dscr("VS", [10, NCTX, HD], BF16)
    GS = dscr("GS", [3 * D, NCTX], BF16)
    OT = dscr("OT", [D, NCTX], BF16)
    dbg = {}
    for name, shape, dt in debug:
        dbg[name] = nc.dram_tensor(name, list(shape), dt, kind="ExternalOutput").ap()

    P = Prog(nc)
    SB = P.stack.enter_context(nc.sbuf_tensor("SB", [128, SB_BYTES], U8))
    PSB = P.stack.enter_context(nc.psum_tensor("PS", [128, 8, 512], F32))
    bank = [T(PSB[:, i, :], excl=True) for i in range(8)]
    A = Arena(SB, SB_BYTES)

    cbf = A.alloc([128, NCBF], BF16)
    cf = A.alloc([128, NCF], F32)
    bar_scr = A.alloc([128, 8], F32)
    P.DMA(cbf.ap, IN("cbf"), writes=[cbf])
    P.DMA(cf.ap, IN("cf32"), writes=[cf])
    ident = cbf[:, C_ID:C_ID + 128]
    ones_bf = cbf[:, C_ONES:C_ONES + 128]
    ntri = cbf[:, C_NTRI:C_NTRI + 128]
    nones = cbf[:, C_NONES:C_NONES + 128]
    persist_mark = A.off

    def new_phase():
        A.off = persist_mark
        P.barrier(bar_scr.ap)

    def gain_col(goff, l, c):
        return cf[:, F_GAIN + goff + 8 * l + c:F_GAIN + goff + 8 * l + c + 1]

    eps_col = cf[:, F_EPS:F_EPS + 1]

    def rmsnorm_group(xg, ntok, goff, l, out_aps, out_t, sq, rstd, nbank):
        for c in range(8):
            P.ACT(sq[:, c, 0:ntok], xg[:, c, 0:ntok], AF.Square, [xg], [sq])
        for c in range(8):
            P.MM(nbank[:, 0:ntok], ones_bf, sq[:, c, 0:ntok], c == 0, c == 7, [sq, cbf], [nbank])
        P.ACT(rstd[:, 0:ntok], nbank[:, 0:ntok], AF.Ln, [nbank, cf], [rstd], scale=1.0 / D, bias=eps_col)
        P.ACT(rstd[:, 0:ntok], rstd[:, 0:ntok], AF.Exp, [rstd], [rstd], scale=-0.5)
        for c in range(8):
            P.STT(out_aps[c], xg[:, c, 0:ntok], gain_col(goff, l, c), rstd[:, 0:ntok], ALU.mult, ALU.mult,
                  [xg, rstd, cf], [out_t])

    def load_w(dram_ap_pcn, stage, wbf, kc, ncols):
        P.DMA(stage[:, 0:kc, 0:ncols], dram_ap_pcn, writes=[stage])
        P.COPY(wbf[:, 0:kc, 0:ncols], stage[:, 0:kc, 0:ncols], [stage], [wbf])

    def phase1(l):
        new_phase()
        src = IN("xT") if l == 0 else XT
        srcv = src.rearrange("(c p) t -> p c t", p=128)
        XTv = XT.rearrange("(c p) t -> p c t", p=128)
        hT = A.alloc([128, 8, NCTX], BF16)
        mark = A.off
        xg = [A.alloc([128, 8, 512], F32) for _ in range(2)]
        sq = A.alloc([128, 8, 512], BF16)
        rstd = [A.alloc([128, 512], F32) for _ in range(2)]
        for g in range(8):
            x_ = xg[g % 2]
            gs = slice(g * 512, (g + 1) * 512)
            P.DMA(x_.ap, srcv[:, :, gs], writes=[x_])
            if l == 0:
                P.DMA(XTv[:, :, gs], x_.ap, reads=[x_])
            rmsnorm_group(x_, 512, G_MIX, l, [hT[:, c, gs] for c in range(8)], hT, sq, rstd[g % 2], bank[7])
        if stop_phase == "p1a_%d" % l:
            P.DMA(dbg["dbg_hT"].rearrange("(c p) t -> p c t", p=128), hT.ap, reads=[hT])
            return
        A.off = mark
        P.barrier(bar_scr.ap)
        stage = [A.alloc([128, 8, 512], F32) for _ in range(2)]
        wbf = [A.alloc([128, 8, 512], BF16) for _ in range(2)]
        ctab = [A.alloc([128, 512], F32) for _ in range(2)]
        stab = [A.alloc([128, 512], F32) for _ in range(2)]
        t1 = [A.alloc([128, 512], F32) for _ in range(2)]
        t2 = [A.alloc([128, 512], F32) for _ in range(2)]
        ob = [A.alloc([128, 512], BF16) for _ in range(6)]
        obi = [0]
        rope_t = IN("rope")
        wv = IN("w_in_ext")[l].rearrange("(c p) n -> p c n", p=128)
        qgroups = list(range(8)) if l == 0 else list(range(4, 8))
        kgroups = list(range(8))
        blocks = [
            (0, [("rope", "q", QA, 0, 0), ("rope", "q", QA, 2, 256)]),
            (1, [("rope", "k", KA, 0, 0), ("rope", "k", KA, 2, 256)]),
            (2, [("rope", "q", QB, 0, 0), ("rope", "q", QB, 2, 256)]),
            (3, [("rope", "q", QB, 4, 0), ("rope", "q", QB, 6, 256)]),
            (4, [("rope", "k", KB, 0, 0), ("plain", "q", QC, 0, 256), ("plain", "q", QC, 2, 384)]),
            (5, [("plain", "k", KC, 0, 0), ("plain", "k", KC, 2, 128), ("v", "k", None, 0, 256, 256)]),
            (6, [("v", "k", None, 4, 0, 384)]),
        ] + [(7 + i, [("gate", "q", None, 4 * i + j, 128 * j) for j in range(4)]) for i in range(6)]

        def issue_load(bi):
            blk = blocks[bi][0]
            load_w(wv[:, :, blk * 512:(blk + 1) * 512], stage[bi % 2], wbf[bi % 2], 8, 512)

        pb = [0]

        def nb():
            pb[0] = (pb[0] + 1) % 6
            return bank[pb[0]]

        def nob():
            o = ob[obi[0] % 6]
            obi[0] += 1
            return o

        import os
        if os.environ.get("P1_ONLY"):
            sel = [int(x) for x in os.environ["P1_ONLY"].split(",")]
            blocks = [b for b in blocks if b[0] in sel]
            A_ = None
        issue_load(0)
        for bi, (blk, jobs) in enumerate(blocks):
            if bi + 1 < len(blocks):
                issue_load(bi + 1)
            W = wbf[bi % 2]
            has_k = any(j[1] == "k" for j in jobs)
            groups = kgroups if has_k else qgroups
            def rope_tabs(gidx):
                g_ = groups[gidx]
                kinds = sorted(set(j[1] for j in jobs if j[0] == "rope" and (j[1] == "k" or g_ in qgroups)))
                out_ = {}
                for kind in kinds:
                    ti = 0 if kind == "q" else 2
                    ct, st_ = ctab[gidx % 2], stab[gidx % 2]
                    gs_ = slice(g_ * 512, (g_ + 1) * 512)
                    for hf_ in range(2):
                        P.DMA(ct[64 * hf_:64 * hf_ + 64, :], rope_t[ti, :, gs_], writes=[ct])
                        P.DMA(st_[64 * hf_:64 * hf_ + 64, :], rope_t[ti + 1, :, gs_], writes=[st_])
                    out_[kind] = (ct, st_)
                return out_

            tb_next = rope_tabs(0)
            for gidx, g in enumerate(groups):
                gs = slice(g * 512, (g + 1) * 512)
                do_q = g in qgroups
                tb = tb_next
                if gidx + 1 < len(groups):
                    tb_next = rope_tabs(gidx + 1)
                for job in jobs:
                    typ, kind = job[0], job[1]
                    if kind == "q" and not do_q:
                        continue
                    if typ == "rope":
                        _, _, dst, h, c0 = job
                        bx, by = nb(), nb()
                        for c in range(8):
                            P.MM(bx.ap, W[:, c, c0:c0 + 128], hT[:, c, gs], c == 0, c == 7, [W, hT], [bx])
                        for c in range(8):
                            P.MM(by.ap, W[:, c, c0 + 128:c0 + 256], hT[:, c, gs], c == 0, c == 7, [W, hT], [by])
                        ct, st_ = tb[kind]
                        a1, a2 = t1[obi[0] % 2], t2[obi[0] % 2]
                        o = nob()
                        P.TT(a1.ap, bx.ap, ct.ap, ALU.mult, [bx, ct], [a1])
                        P.TT(a2.ap, by.ap, st_.ap, ALU.mult, [by, st_], [a2])
                        P.TT(o.ap, a1.ap, a2.ap, ALU.add, [a1, a2], [o], eng="pool")
                        P.DMA(dst[h:h + 2, :, gs].rearrange("h d t -> (h d) t"), o.ap, reads=[o])
                    elif typ == "plain":
                        _, _, dst, h, c0 = job
                        bx = nb()
                        for c in range(8):
                            P.MM(bx.ap, W[:, c, c0:c0 + 128], hT[:, c, gs], c == 0, c == 7, [W, hT], [bx])
                        o = nob()
                        P.ACT(o.ap, bx.ap, AF.Copy, [bx], [o], scale=(0.125 if kind == "q" else 1.0))
                        P.DMA(dst[h:h + 2, :, gs].rearrange("h d t -> (h d) t"), o.ap, reads=[o])
                    elif typ == "gate":
                        _, _, _, jc, c0 = job
                        bx = nb()
                        for c in range(8):
                            P.MM(bx.ap, W[:, c, c0:c0 + 128], hT[:, c, gs], c == 0, c == 7, [W, hT], [bx])
                        o = nob()
                        P.ACT(o.ap, bx.ap, AF.Sigmoid, [bx], [o])
                        P.DMA(GS[jc * 128:(jc + 1) * 128, gs], o.ap, reads=[o])
                    elif typ == "v":
                        _, _, _, h0, c0, ncol = job
                        nh = ncol // 64
                        for tt in range(4):
                            tok = slice(g * 512 + tt * 128, g * 512 + (tt + 1) * 128)
                            bx = nb()
                            for c in range(8):
                                P.MM(bx[:, 0:ncol], hT[:, c, tok], W[:, c, c0:c0 + ncol], c == 0, c == 7, [W, hT], [bx])
                            o = nob()
                            P.ACT(o[:, 0:ncol], bx[:, 0:ncol], AF.Copy, [bx], [o])
                            P.DMA(VS[h0:h0 + nh, tok, :].rearrange("h t d -> t h d"),
                                  o[:, 0:ncol].rearrange("t (h d) -> t h d", d=64), reads=[o])


    def phase2(l):
        new_phase()
        rows = IN("rows_bf")
        q0 = 0 if l == 0 else HALF
        g0 = q0 // 512
        t0_ = q0 // 128
        Kt = [A.alloc([80, NCTX], BF16) for _ in range(2)]
        Qt = [A.alloc([80, NCTX], BF16) for _ in range(2)]
        Vt = [A.alloc([128, 32, 128], BF16) for _ in range(2)]
        dcp = [A.alloc([128, 512], F32) for _ in range(3)]
        dlo = [A.alloc([64, 512], F32) for _ in range(3)]
        esb = [A.alloc([128, 512], F32) for _ in range(2)]
        spb = [A.alloc([128, 512], BF16) for _ in range(2)]
        argb = [A.alloc([128, 512], F32) for _ in range(2)]
        wsb = [A.alloc([128, 512], BF16) for _ in range(3)]
        Rsb = A.alloc([128, 512], F32)
        rden = A.alloc([64, 512], F32)
        osb = [A.alloc([64, 512], BF16) for _ in range(3)]
        gm = A.alloc([128, 512], F32)
        usb = A.alloc([128, 512], F32)
        top8 = A.alloc([128, 32, 8], F32)
        selpad = A.alloc([128, 32, 80], BF16)
        km = A.alloc([64, 16], F32)
        kmb = A.alloc([64, 16], BF16)
        esink = A.alloc([128, 16], F32)
        cnt = {"w": 0, "o": 0, "z": 0, "r": 0, "bo": 0, "sp": 0, "o2": 0, "dc": 0, "dl": 0}

        def nxt(key, lst):
            t_ = lst[cnt[key] % len(lst)]
            cnt[key] += 1
            return t_

        def load_head(Ksrc, Qsrc, vh, slot, krows):
            K_, Q_, V_ = Kt[slot], Qt[slot], Vt[slot]
            P.DMA(K_[0:64, :], Ksrc, writes=[K_])
            P.DMA(Q_[0:64, q0:NCTX], Qsrc[:, q0:NCTX], writes=[Q_])
            for i in range(4):
                P.DMA(V_[:, 8 * i:8 * i + 8, 0:64], VS[vh, 1024 * i:1024 * (i + 1), :].rearrange("(t p) d -> p t d", p=128),
                      writes=[V_])
            return K_, Q_, V_

        def store_o(bo_, bden_, row0, gs, extra_col=None):
            if bden_ is not None:
                if bden_ == "merged":
                    dc, dl = nxt("dc", dcp), nxt("dl", dlo)
                    P.ACT(dc[64:128, :], bo_[64:128, :], AF.Copy, [bo_], [dc])
                    P.DMA(dl.ap, dc[64:128, :], reads=[dc], writes=[dl])
                    dsrc, dres = dl.ap, dl
                else:
                    dsrc, dres = bden_[0:64, :], bden_
                if extra_col is not None:
                    P.TS(rden.ap, dsrc, extra_col, None, ALU.add, None, [dres, esink], [rden])
                    P.dve(lambda e: e.reciprocal(out=rden.ap, in_=rden.ap), [rden], [rden])
                else:
                    P.dve(lambda e: e.reciprocal(out=rden.ap, in_=dsrc), [dres], [rden])
                o = nxt("o", osb)
                P.TT(o.ap, bo_[0:64, :], rden.ap, ALU.mult, [bo_, rden], [o])
            else:
                o = nxt("o", osb)
                P.ACT(o.ap, bo_[0:64, :], AF.Copy, [bo_], [o])
            P.DMA(OT[row0:row0 + 64, gs], o.ap, reads=[o])

        def run_pipeline(items, nstage):
            n = len(items)
            for i in range(n + nstage - 1):
                for s_ in range(nstage):
                    j = i - s_
                    if 0 <= j < n:
                        items[j][s_]()

        for s_ in range(2):
            P.DMA(Kt[s_][64:80, :], rows[0:16, :], writes=[Kt[s_]])
        P.pool(lambda e: e.memset(selpad.ap, 0.0), [], [selpad])
        for s_ in range(2):
            P.pool(lambda e, s_=s_: e.memset(Vt[s_][:, :, 64:128], 1.0), [], [Vt[s_]])
        nqt = (NCTX - q0) // 128

        def a_load(h):
            load_head(KA[h], QA[h], h, h % 2, 80)

        def a_sel_a(h):
            K_, Q_ = Kt[h % 2], Qt[h % 2]
            P.dve(lambda e: e.tensor_reduce(out=km.ap, in_=K_[0:64, :].rearrange("p (n j) -> p n j", j=256),
                                            axis=AX.X, op=ALU.add), [K_], [km])
            P.ACT(kmb.ap, km.ap, AF.Copy, [km], [kmb])
            bg = bank[7]
            for i in range(nqt):
                ti = t0_ + i
                P.MM(bg[:, 16 * ti:16 * ti + 16], Q_[0:64, ti * 128:(ti + 1) * 128], kmb.ap, True, True, [Q_, kmb], [bg])
            cs = slice(16 * t0_, 512)
            P.TT(gm[:, cs], bg[:, cs], cf[:, F_PB + 16 * t0_:F_PB + 512], ALU.add, [bg, cf], [gm])
            for i in range(nqt):
                ti = t0_ + i
                P.dve(lambda e, ti=ti: e.max(out=top8[:, ti, :], in_=gm[:, 16 * ti:16 * ti + 16]), [gm], [top8])
            for i in range(nqt):
                ti = t0_ + i
                P.STT(usb[:, 16 * ti:16 * ti + 16], gm[:, 16 * ti:16 * ti + 16], top8[:, ti, 2:3],
                      cf[:, F_T1 + 16 * ti:F_T1 + 16 * ti + 16], ALU.is_ge, ALU.mult, [gm, top8, cf], [usb])
            P.TT(selpad[:, t0_:32, 64:80], usb[:, cs].rearrange("p (t n) -> p t n", n=16),
                 cf[:, F_T0 + 16 * t0_:F_T0 + 512].rearrange("p (t n) -> p t n", n=16), ALU.add, [usb, cf], [selpad])

        def a_sel_b(h):
            Q_ = Qt[h % 2]
            for gi in range(nqt // 4):
                bt = bank[5 + (gi % 2)]
                for j in range(4):
                    ti = t0_ + 4 * gi + j
                    P.MM(bt[0:80, 128 * j:128 * j + 128], selpad[:, ti, :], ident, True, True, [selpad, cbf], [bt])
                P.ACT(Q_[64:80, (t0_ + 4 * gi) * 128:(t0_ + 4 * gi + 4) * 128], bt[64:80, :], AF.Copy, [bt], [Q_])

        a_load(0)
        a_sel_a(0)
        a_sel_b(0)
        items = []
        for h in range(4):
            K_, Q_, V_ = Kt[h % 2], Qt[h % 2], Vt[h % 2]
            hp = [(G, J) for G in range(g0, 8) for J in range(4 * G + 4)]
            for pi, (G, J) in enumerate(hp):
                st = {}

                def s0(h=h, pi=pi, G=G, J=J, K_=K_, Q_=Q_, st=st, n=len(hp)):
                    if pi == 4 and h + 1 < 4:
                        a_load(h + 1)
                    if pi == n // 2 and h + 1 < 4:
                        a_sel_a(h + 1)
                    if pi == n - 1 and h + 1 < 4:
                        a_sel_b(h + 1)
                    a = J - 4 * G
                    bs = bank[cnt["z"] % 3]
                    cnt["z"] += 1
                    st["bs"] = bs
                    gs = slice(G * 512, (G + 1) * 512)
                    P.MM(bs.ap, K_[0:80, J * 128:(J + 1) * 128], Q_[0:80, gs], True, a < 0, [K_, Q_], [bs])
                    if a >= 0:
                        P.MM(bs.ap, ident, cbf[:, C_CBA + a * 512:C_CBA + (a + 1) * 512], False, True, [cbf], [bs])

                def s1(st=st):
                    w_ = nxt("w", wsb)
                    st["w"] = w_
                    P.ACT(w_.ap, st["bs"].ap, AF.Exp, [st["bs"]], [w_])

                def s2(h=h, pi=pi, G=G, J=J, V_=V_, st=st, n=len(hp)):
                    nJ = 4 * G + 4
                    bo_ = bank[3 + (G % 2)]
                    w_ = st["w"]
                    P.MM(bo_.ap, V_[:, J, :], w_.ap, J == 0, J == nJ - 1, [V_, w_], [bo_])
                    if J == nJ - 1:
                        store_o(bo_, "merged", h * 64, slice(G * 512, (G + 1) * 512))

                items.append([s0, s1, s2])
        run_pipeline(items, 3)

        P.barrier(bar_scr.ap)
        for s_ in range(2):
            P.DMA(Kt[s_][64:65, :], rows[16:17, :], writes=[Kt[s_]])
            P.DMA(Qt[s_][64:65, :], rows[17:18, :], writes=[Qt[s_]])
        e2b = [A.alloc([128, 2, 512], F32) for _ in range(2)]
        sp2b = [A.alloc([128, 2, 512], BF16) for _ in range(3)]
        ar2b = [A.alloc([128, 2, 512], F32) for _ in range(2)]
        w2b = [A.alloc([128, 2, 512], BF16) for _ in range(3)]

        def c_load(h):
            load_head(KC[h], QC[h], 4 + h, h % 2, 65)

        c_load(0)
        items = []
        cz = [0]
        for h in range(4):
            K_, Q_, V_ = Kt[h % 2], Qt[h % 2], Vt[h % 2]
            hp = [(G, jp) for G in range(g0, 8) for jp in range(2 * G + 2)]
            for pi, (G, jp) in enumerate(hp):
                st = {}
                top = 4 * G + 3
                Ja, Jb = top - 2 * jp, top - 2 * jp - 1

                def s0(h=h, pi=pi, G=G, Ja=Ja, Jb=Jb, K_=K_, Q_=Q_, st=st):
                    if pi == 4 and h + 1 < 4:
                        c_load(h + 1)
                    gs = slice(G * 512, (G + 1) * 512)
                    slot = cz[0] % 2
                    cz[0] += 1
                    bA, bB = bank[2 * slot], bank[2 * slot + 1]
                    st["b"] = (bA, bB, PSB[:, 2 * slot:2 * slot + 2, :])
                    for (b_, J) in ((bA, Ja), (bB, Jb)):
                        a = J - 4 * G
                        P.MM(b_.ap, K_[0:65, J * 128:(J + 1) * 128], Q_[0:65, gs], True, False, [K_, Q_], [b_])
                        if a >= 0:
                            P.MM(b_.ap, ident, cbf[:, C_CBC + a * 512:C_CBC + (a + 1) * 512], False, False, [cbf], [b_])
                    e_ = nxt("r", e2b)
                    sp_ = nxt("sp", sp2b)
                    st["sp"] = sp_
                    P.ACT(e_.ap, st["b"][2], AF.Exp, [bA, bB], [e_])
                    P.ACT(sp_.ap, e_.ap, AF.Ln, [e_, cf], [sp_], bias=cf[:, F_ONE:F_ONE + 1])

                def s1(jp=jp, Jb=Jb, st=st):
                    bA, bB, bAB = st["b"]
                    sp_ = st["sp"]
                    P.MM(bA.ap, ntri, sp_[:, 0, :], False, True, [cbf, sp_], [bA])
                    P.MM(bB.ap, ntri, sp_[:, 1, :], False, False, [cbf, sp_], [bB])
                    P.MM(bB.ap, nones, sp_[:, 0, :], False, True, [cbf, sp_], [bB])
                    if Jb > 0:
                        br = bank[4 + (jp % 2)]
                        P.MM(br.ap, ones_bf, sp_[:, 0, :], True, False, [cbf, sp_], [br])
                        P.MM(br.ap, ones_bf, sp_[:, 1, :], False, True, [cbf, sp_], [br])
                    w_ = nxt("w", w2b)
                    st["w"] = w_
                    if jp == 0:
                        P.ACT(w_.ap, bAB, AF.Exp, [bA, bB], [w_])
                    else:
                        ar = nxt("o2", ar2b)
                        P.TT(ar[:, 0, :], bA.ap, Rsb.ap, ALU.subtract, [bA, Rsb], [ar])
                        P.TT(ar[:, 1, :], bB.ap, Rsb.ap, ALU.subtract, [bB, Rsb], [ar])
                        P.ACT(w_.ap, ar.ap, AF.Exp, [ar], [w_])
                    if Jb > 0:
                        if jp == 0:
                            P.COPY(Rsb.ap, br.ap, [br], [Rsb])
                        else:
                            P.TT(Rsb.ap, br.ap, Rsb.ap, ALU.add, [br, Rsb], [Rsb])

                def s2(h=h, G=G, jp=jp, Ja=Ja, Jb=Jb, V_=V_, st=st):
                    bo_ = bank[6 + (G % 2)]
                    w_ = st["w"]
                    P.MM(bo_[0:64, :], V_[:, Ja, 0:64], w_[:, 0, :], jp == 0, False, [V_, w_], [bo_])
                    P.MM(bo_[0:64, :], V_[:, Jb, 0:64], w_[:, 1, :], False, Jb == 0, [V_, w_], [bo_])
                    if Jb == 0:
                        store_o(bo_, None, 768 + h * 64, slice(G * 512, (G + 1) * 512))

                items.append([s0, s1, s2])
        run_pipeline(items, 3)

        P.barrier(bar_scr.ap)
        P.ACT(esink.ap, cf[:, F_SINK:F_SINK + 16], AF.Exp, [cf], [esink])

        def b_load_kv(kv):
            K_, V_ = Kt[kv], Vt[kv]
            P.DMA(K_[0:64, :], KB[kv], writes=[K_])
            for i in range(4):
                P.DMA(V_[:, 8 * i:8 * i + 8, 0:64], VS[8 + kv, 1024 * i:1024 * (i + 1), :].rearrange("(t p) d -> p t d", p=128),
                      writes=[V_])

        def b_load_q(hq):
            Q_ = Qt[hq % 2]
            P.DMA(Q_[0:64, q0:NCTX], QB[hq][:, q0:NCTX], writes=[Q_])

        b_load_kv(0)
        b_load_kv(1)
        b_load_q(0)
        items = []
        for hq in range(8):
            kv = hq // 4
            K_, V_, Q_ = Kt[kv], Vt[kv], Qt[hq % 2]
            tiles = list(range(4 * g0, 32))
            for pi, I in enumerate(tiles):
                st = {}

                def s0(hq=hq, pi=pi, I=I, K_=K_, Q_=Q_, st=st):
                    if pi == 4 and hq + 1 < 8:
                        b_load_q(hq + 1)
                    qs = slice(I * 128, (I + 1) * 128)
                    bs = bank[cnt["z"] % 3]
                    cnt["z"] += 1
                    st["bs"] = bs
                    if I > 0:
                        P.MM(bs[:, 0:128], K_[0:65, (I - 1) * 128:I * 128], Q_[0:65, qs], True, False, [K_, Q_], [bs])
                        P.MM(bs[:, 0:128], ident, cbf[:, C_CBB + 128:C_CBB + 256], False, True, [cbf], [bs])
                    P.MM(bs[:, 128:256], K_[0:65, qs], Q_[0:65, qs], True, False, [K_, Q_], [bs])
                    P.MM(bs[:, 128:256], ident, cbf[:, C_CBB:C_CBB + 128], False, True, [cbf], [bs])

                def s1(I=I, st=st):
                    w_ = nxt("w", wsb)
                    st["w"] = w_
                    c0 = 0 if I > 0 else 128
                    P.ACT(w_[:, c0:256], st["bs"][:, c0:256], AF.Exp, [st["bs"]], [w_])

                def s2(hq=hq, I=I, V_=V_, st=st):
                    G, j = I // 4, I % 4
                    bo_, bden_ = bank[3 + 2 * (G % 2)], bank[4 + 2 * (G % 2)]
                    w_ = st["w"]
                    oc = slice(128 * j, 128 * j + 128)
                    if I > 0:
                        P.MM(bo_[0:64, oc], V_[:, I - 1, 0:64], w_[:, 0:128], True, False, [V_, w_], [bo_])
                        P.MM(bden_[0:64, oc], ones_bf[:, 0:64], w_[:, 0:128], True, False, [cbf, w_], [bden_])
                    P.MM(bo_[0:64, oc], V_[:, I, 0:64], w_[:, 128:256], I == 0, True, [V_, w_], [bo_])
                    P.MM(bden_[0:64, oc], ones_bf[:, 0:64], w_[:, 128:256], I == 0, True, [cbf, w_], [bden_])
                    if j == 3:
                        store_o(bo_, bden_, 256 + hq * 64, slice(G * 512, (G + 1) * 512),
                                extra_col=esink[0:64, 8 * l + hq:8 * l + hq + 1])

                items.append([s0, s1, s2])
        run_pipeline(items, 3)

    def phase3(l):
        new_phase()
        XTv = XT.rearrange("(c p) t -> p c t", p=128)
        OTv = OT.rearrange("(c p) t -> p c t", p=128)
        GSv = GS.rearrange("(c p) t -> p c t", p=128)
        wproj = A.alloc([128, 8, 1024], BF16)
        wmix = A.alloc([128, 8, 1024], BF16)
        wxq = A.alloc([128, 8, 512], BF16)
        wxo = A.alloc([128, 4, 1024], BF16)
        KmT = A.alloc([128, 4, 256], BF16)
        Vm = A.alloc([128, 2, 512], BF16)
        mark = A.off
        stage = [A.alloc([128, 8, 512], F32) for _ in range(2)]
        wxkv = A.alloc([128, 8, 1024], BF16)
        memx = A.alloc([128, 8, 256], F32)
        memh = A.alloc([128, 8, 256], BF16)
        sq = A.alloc([128, 8, 512], BF16)
        rstd = A.alloc([128, 512], F32)
        si = [0]

        def ld(dram_pcn, dst_ap, dst_t, kc, ncols):
            st_ = stage[si[0] % 2]
            si[0] += 1
            P.DMA(st_[:, 0:kc, 0:ncols], dram_pcn, writes=[st_])
            P.COPY(dst_ap, st_[:, 0:kc, 0:ncols], [st_], [dst_t])

        wp = IN("w_proj")[l].rearrange("(c p) n -> p c n", p=128)
        wm = IN("w_mix_out")[l].rearrange("(c p) n -> p c n", p=128)
        wq = IN("w_xq")[l].rearrange("(c p) n -> p c n", p=128)
        wkv = IN("w_xkv")[l].rearrange("(c p) n -> p c n", p=128)
        wo = IN("w_xo")[l].rearrange("(c p) n -> p c n", p=128)
        for hlf in range(2):
            cs = slice(512 * hlf, 512 * hlf + 512)
            ld(wkv[:, :, cs], wxkv[:, :, cs], wxkv, 8, 512)
        for hlf in range(2):
            cs = slice(512 * hlf, 512 * hlf + 512)
            ld(wp[:, :, cs], wproj[:, :, cs], wproj, 8, 512)
            ld(wm[:, :, cs], wmix[:, :, cs], wmix, 8, 512)
            ld(wo[:, :, cs], wxo[:, :, cs], wxo, 4, 512)
        ld(wq, wxq.ap, wxq, 8, 512)
        P.DMA(memx.ap, IN("memT").rearrange("(c p) t -> p c t", p=128), writes=[memx])
        rmsnorm_group(memx, 256, G_MEM, l, [memh[:, c, :] for c in range(8)], memh, sq, rstd, bank[7])
        for hx in range(4):
            b_ = bank[hx % 2]
            for c in range(8):
                P.MM(b_[:, 0:256], wxkv[:, c, hx * 128:(hx + 1) * 128], memh[:, c, :], c == 0, c == 7, [wxkv, memh], [b_])
            P.ACT(KmT[:, hx, :], b_[:, 0:256], AF.Copy, [b_], [KmT])
        for mt in range(2):
            b_ = bank[2 + mt]
            for c in range(8):
                P.MM(b_.ap, memh[:, c, mt * 128:(mt + 1) * 128], wxkv[:, c, 512:1024], c == 0, c == 7, [wxkv, memh], [b_])
            P.ACT(Vm[:, mt, :], b_.ap, AF.Copy, [b_], [Vm])
        P.barrier(bar_scr.ap)
        A.off = mark
        xg = [A.alloc([128, 8, 512], F32) for _ in range(2)]
        ot = [A.alloc([128, 8, 512], BF16) for _ in range(2)]
        gt = A.alloc([128, 24, 512], BF16)
        mT = A.alloc([128, 8, 512], BF16)
        h2 = A.alloc([128, 8, 512], BF16)
        sq = A.alloc([128, 8, 512], BF16)
        rstd = A.alloc([128, 512], F32)
        qx = A.alloc([128, 4, 512], BF16)
        oc = A.alloc([128, 4, 512], BF16)
        pT = [A.alloc([128, 512], BF16) for _ in range(3)]
        tmpb = [A.alloc([128, 512], BF16) for _ in range(6)]
        rdn = A.alloc([128, 512], F32)
        groups = list(range(8)) if l == 0 else list(range(4, 8))
        xscale = 128 ** -0.5

        def loads(gi):
            g = groups[gi]
            gs = slice(g * 512, (g + 1) * 512)
            P.DMA(xg[gi % 2].ap, XTv[:, :, gs], writes=[xg[gi % 2]])
            P.DMA(ot[gi % 2].ap, OTv[:, :, gs], writes=[ot[gi % 2]])

        loads(0)
        pc = [0]
        for gi, g in enumerate(groups):
            gs = slice(g * 512, (g + 1) * 512)
            X, O_ = xg[gi % 2], ot[gi % 2]
            if gi == 0:
                for part in range(3):
                    P.DMA(gt[:, 8 * part:8 * part + 8, :], GSv[:, 8 * part:8 * part + 8, gs], writes=[gt])
            if gi + 1 < len(groups):
                loads(gi + 1)
            for j in range(8):
                js = slice(j * 128, (j + 1) * 128)
                b0, b1, b2 = bank[0 + 3 * (j % 2)], bank[1 + 3 * (j % 2)], bank[2 + 3 * (j % 2)]
                for c in range(0, 2):
                    P.MM(b0.ap, wproj[:, c, js], O_[:, c, :], c == 0, c == 1, [wproj, O_], [b0])
                for c in range(2, 6):
                    P.MM(b1.ap, wproj[:, c, js], O_[:, c, :], c == 2, c == 5, [wproj, O_], [b1])
                for c in range(6, 8):
                    P.MM(b2.ap, wproj[:, c, js], O_[:, c, :], c == 6, c == 7, [wproj, O_], [b2])
                ta, tb_, tc = tmpb[(3 * j) % 6], tmpb[(3 * j + 1) % 6], tmpb[(3 * j + 2) % 6]
                P.TT(ta.ap, b0.ap, gt[:, j, :], ALU.mult, [b0, gt], [ta])
                P.TT(tb_.ap, b1.ap, gt[:, 8 + j, :], ALU.mult, [b1, gt], [tb_])
                P.TT(tc.ap, b2.ap, gt[:, 16 + j, :], ALU.mult, [b2, gt], [tc])
                P.TT(ta.ap, ta.ap, tb_.ap, ALU.add, [ta, tb_], [ta])
                P.TT(mT[:, j, :], ta.ap, tc.ap, ALU.add, [ta, tc], [mT])
            if gi + 1 < len(groups):
                gn = groups[gi + 1]
                for part in range(3):
                    P.DMA(gt[:, 8 * part:8 * part + 8, :], GSv[:, 8 * part:8 * part + 8, gn * 512:(gn + 1) * 512], writes=[gt])
            for j in range(8):
                js = slice(j * 128, (j + 1) * 128)
                b_ = bank[j % 4]
                for c in range(8):
                    P.MM(b_.ap, wmix[:, c, js], mT[:, c, :], c == 0, c == 7, [wmix, mT], [b_])
                P.TT(X[:, j, :], b_.ap, X[:, j, :], ALU.add, [b_, X], [X])
            rmsnorm_group(X, 512, G_CROSS, l, [h2[:, c, :] for c in range(8)], h2, sq, rstd, bank[7])
            for hx in range(4):
                b_ = bank[hx % 4]
                for c in range(8):
                    P.MM(b_.ap, wxq[:, c, hx * 128:(hx + 1) * 128], h2[:, c, :], c == 0, c == 7, [wxq, h2], [b_])
                P.ACT(qx[:, hx, :], b_.ap, AF.Copy, [b_], [qx])
            for hx in range(4):
                bo_, bd_ = bank[4 + 2 * (hx % 2)], bank[5 + 2 * (hx % 2)]
                for mt in range(2):
                    bs = bank[(2 * hx + mt) % 4]
                    P.MM(bs.ap, KmT[:, hx, mt * 128:(mt + 1) * 128], qx[:, hx, :], True, True, [KmT, qx], [bs])
                    p_ = pT[pc[0] % 3]
                    pc[0] += 1
                    P.ACT(p_.ap, bs.ap, AF.Exp, [bs], [p_], scale=xscale)
                    P.MM(bo_.ap, Vm[:, mt, hx * 128:(hx + 1) * 128], p_.ap, mt == 0, mt == 1, [Vm, p_], [bo_])
                    P.MM(bd_.ap, ones_bf, p_.ap, mt == 0, mt == 1, [cbf, p_], [bd_])
                P.dve(lambda e, bd_=bd_: e.reciprocal(out=rdn.ap, in_=bd_.ap), [bd_], [rdn])
                P.TT(oc[:, hx, :], bo_.ap, rdn.ap, ALU.mult, [bo_, rdn], [oc])
            for j in range(8):
                js = slice(j * 128, (j + 1) * 128)
                b_ = bank[j % 4]
                for hx in range(4):
                    P.MM(b_.ap, wxo[:, hx, js], oc[:, hx, :], hx == 0, hx == 3, [wxo, oc], [b_])
                P.TT(X[:, j, :], b_.ap, X[:, j, :], ALU.add, [b_, X], [X])
            P.DMA(XTv[:, :, gs], X.ap, reads=[X])

    def phase4(l, last):
        new_phase()
        XTv = XT.rearrange("(c p) t -> p c t", p=128)
        moe = (l % 2 == 1)
        li = l // 2
        FB = 256
        NFC = FB // 128
        if moe:
            F_, nexp = D_FFE, NE
            wgs = [IN("moe_gate")[li, e_].rearrange("(c p) n -> p c n", p=128) for e_ in range(NE)]
            wus = [IN("moe_up")[li, e_].rearrange("(c p) n -> p c n", p=128) for e_ in range(NE)]
            wds = [IN("moe_down")[li, e_].rearrange("(c p) n -> p c n", p=128) for e_ in range(NE)]
        else:
            F_, nexp = D_FF, 1
            wgs = [IN("ffn_gate")[li].rearrange("(c p) n -> p c n", p=128)]
            wus = [IN("ffn_up")[li].rearrange("(c p) n -> p c n", p=128)]
            wds = [IN("ffn_down")[li].rearrange("(c p) n -> p c n", p=128)]
        nfb = F_ // FB
        yacc = A.alloc([128, 8, 2048], F32)
        h3 = A.alloc([128, 8, 2048], BF16)
        stage = [A.alloc([128, 8, FB], F32) for _ in range(2)]
        wg = [A.alloc([128, 8, FB], BF16) for _ in range(2)]
        wu = [A.alloc([128, 8, FB], BF16) for _ in range(2)]
        wd = [A.alloc([128, NFC, 1024], BF16) for _ in range(2)]
        aT = [A.alloc([128, NFC, 512], BF16) for _ in range(3)]
        sgs = [A.alloc([128, 512], BF16) for _ in range(2)]
        cbs = [A.alloc([128, 512], BF16) for _ in range(4)]
        sq = A.alloc([128, 8, 512], BF16)
        rstd = A.alloc([128, 512], F32)
        hf = A.alloc([128, 8, 512], F32)
        wr = A.alloc([128, 8, 8], F32)
        lg = A.alloc([128, 16, 8], F32)
        top8 = A.alloc([128, 16, 8], F32)
        rt = [A.alloc([128, 16], F32) for _ in range(3)]
        comb = A.alloc([128, 16, 8], F32)
        ctmp = A.alloc([128, 8], F32)
        dg = [A.alloc([128, 128], F32) for _ in range(2)]
        ident32 = cf[:, F_ID:F_ID + 128]
        ones32 = cf[:, F_ONES32:F_ONES32 + 128]
        sgroups = [0, 1] if l == 0 else [1]
        if moe:
            P.DMA(wr.ap, IN("moe_router")[li].rearrange("(c p) n -> p c n", p=128), writes=[wr])
        for sgi in sgroups:
            t0_ = sgi * 2048
            for g in range(4):
                P.DMA(yacc[:, :, g * 512:(g + 1) * 512], XTv[:, :, t0_ + g * 512:t0_ + (g + 1) * 512], writes=[yacc])
            for g in range(4):
                gl = slice(g * 512, (g + 1) * 512)
                xv = T(yacc.ap[:, :, gl], res=yacc.res)
                if moe:
                    rmsnorm_group(xv, 512, G_FFN, l, [hf[:, c, :] for c in range(8)], hf, sq, rstd, bank[7])
                    for c in range(8):
                        P.ACT(h3[:, c, gl], hf[:, c, :], AF.Copy, [hf], [h3])
                    for tt in range(4):
                        b_ = bank[6]
                        ti = 4 * g + tt
                        for c in range(8):
                            P.MM(b_[:, 8 * tt:8 * tt + 8], hf[:, c, tt * 128:(tt + 1) * 128], wr[:, c, :], c == 0, c == 7,
                                 [hf, wr], [b_])
                    P.COPY(lg[:, 4 * g:4 * g + 4, :], bank[6][:, 0:32].rearrange("p (t e) -> p t e", e=8), [bank[6]], [lg])
                else:
                    rmsnorm_group(xv, 512, G_FFN, l, [h3[:, c, gl] for c in range(8)], h3, sq, rstd, bank[7])
            if moe:
                for ti in range(16):
                    P.dve(lambda e, ti=ti: e.max(out=top8[:, ti, :], in_=lg[:, ti, :]), [lg], [top8])
                P.TT(rt[0].ap, top8[:, :, 1], top8[:, :, 0], ALU.subtract, [top8], [rt[0]])
                P.ACT(rt[0].ap, rt[0].ap, AF.Exp, [rt[0]], [rt[0]])
                P.TS(rt[1].ap, rt[0].ap, 1.0, None, ALU.add, None, [rt[0]], [rt[1]])
                P.dve(lambda e: e.reciprocal(out=rt[1].ap, in_=rt[1].ap), [rt[1]], [rt[1]])
                P.TT(rt[2].ap, rt[0].ap, rt[1].ap, ALU.mult, [rt[0], rt[1]], [rt[2]])
                for ti in range(16):
                    P.TS(comb[:, ti, :], lg[:, ti, :], top8[:, ti, 0:1], rt[1][:, ti:ti + 1], ALU.is_equal, ALU.mult,
                         [lg, top8, rt[1]], [comb])
                    P.TS(ctmp.ap, lg[:, ti, :], top8[:, ti, 1:2], rt[2][:, ti:ti + 1], ALU.is_equal, ALU.mult,
                         [lg, top8, rt[2]], [ctmp])
                    P.TT(comb[:, ti, :], comb[:, ti, :], ctmp.ap, ALU.add, [comb, ctmp], [comb])
            steps = [(e_, fb) for e_ in range(nexp) for fb in range(nfb)]
            sti = [0]

            def issue(k, part):
                e_, fb = steps[k]
                fs = slice(fb * FB, (fb + 1) * FB)
                (src, dst, kc, nco) = ((wgs[e_][:, :, fs], wg[k % 2], 8, FB), (wus[e_][:, :, fs], wu[k % 2], 8, FB),
                                       (wds[e_][:, NFC * fb:NFC * fb + NFC, :], wd[k % 2], NFC, 1024))[part]
                st_ = stage[sti[0] % 2]
                sti[0] += 1
                stv = T(st_.ap.rearrange("p a b -> p (a b)").rearrange("p (a b) -> p a b", b=nco), res=st_.res)
                P.DMA(stv[:, 0:kc, :], src, writes=[st_])
                P.ACT(dst.ap, stv[:, 0:kc, :], AF.Copy, [st_], [dst])

            for part_ in range(3):
                issue(0, part_)
            ai = [0]
            bdi = [0]
            bdbanks = [bank[4], bank[5], bank[6], bank[7]]
            items = []
            for k, (e_, fb) in enumerate(steps):
                for g in range(4):
                    st = {}

                    def gu(fc, k, g, st):
                        G_, U_ = wg[k % 2], wu[k % 2]
                        gl = slice(g * 512, (g + 1) * 512)
                        a_ = st["a"]
                        bg_, bu_ = bank[2 * (fc % 2)], bank[2 * (fc % 2) + 1]
                        for c in range(8):
                            P.MM(bg_.ap, G_[:, c, fc * 128:(fc + 1) * 128], h3[:, c, gl], c == 0, c == 7, [G_, h3], [bg_])
                        for c in range(8):
                            P.MM(bu_.ap, U_[:, c, fc * 128:(fc + 1) * 128], h3[:, c, gl], c == 0, c == 7, [U_, h3], [bu_])
                        s_ = sgs[fc % 2]
                        P.ACT(s_.ap, bg_.ap, AF.Silu, [bg_], [s_])
                        if moe:
                            P.TT(s_.ap, s_.ap, cbs[g].ap, ALU.mult, [s_, cbs[g]], [s_])
                        P.TT(a_[:, fc, :], bu_.ap, s_.ap, ALU.mult, [bu_, s_], [a_])

                    def s0a(k=k, e_=e_, fb=fb, g=g, st=st):
                        if g >= 1 and k + 1 < len(steps):
                            issue(k + 1, g - 1)
                        if moe and fb == 0:
                            cb_ = cbs[g]
                            for tt in range(4):
                                d_ = dg[tt % 2]
                                P.TS(d_.ap, ident32, comb[:, 4 * g + tt, e_:e_ + 1], None, ALU.mult, None, [cf, comb], [d_])
                                P.MM(bank[6][:, tt * 128:(tt + 1) * 128], ones32, d_.ap, True, True, [cf, d_], [bank[6]])
                            P.ACT(cb_.ap, bank[6].ap, AF.Copy, [bank[6]], [cb_])
                        st["a"] = aT[ai[0] % 3]
                        ai[0] += 1
                        gu(0, k, g, st)

                    def s0b(k=k, g=g, st=st):
                        for fc in range(1, NFC):
                            gu(fc, k, g, st)

                    def down(j0, j1, k, g, st):
                        D_ = wd[k % 2]
                        a_ = st["a"]
                        gl = slice(g * 512, (g + 1) * 512)
                        for j in range(j0, j1):
                            bd_ = bdbanks[bdi[0] % 4]
                            bdi[0] += 1
                            for fc in range(NFC):
                                P.MM(bd_.ap, D_[:, fc, j * 128:(j + 1) * 128], a_[:, fc, :], fc == 0, fc == NFC - 1, [D_, a_], [bd_])
                            P.TT(yacc[:, j, gl], bd_.ap, yacc[:, j, gl], ALU.add, [bd_, yacc], [yacc])

                    def s1a(k=k, g=g, st=st):
                        down(0, 4, k, g, st)

                    def s1b(k=k, g=g, st=st):
                        down(4, 8, k, g, st)

                    items.append([s0a, s0b, s1a, s1b])
            n_it = len(items)
            for i in range(n_it + 1):
                if i < n_it:
                    items[i][0]()
                if i >= 1:
                    items[i - 1][2]()
                if i < n_it:
                    items[i][1]()
                if i >= 1:
                    items[i - 1][3]()
            if not last:
                for g in range(4):
                    P.DMA(XTv[:, :, t0_ + g * 512:t0_ + (g + 1) * 512], yacc[:, :, g * 512:(g + 1) * 512], reads=[yacc])
            else:
                oT = out_T.rearrange("(c p) t -> p c t", p=128)
                for g in range(4):
                    gl = slice(g * 512, (g + 1) * 512)
                    xv = T(yacc.ap[:, :, gl], res=yacc.res)
                    rmsnorm_group(xv, 512, G_FINAL, 0, [hf[:, c, :] for c in range(8)], hf, sq, rstd, bank[7])
                    P.DMA(oT[:, :, gl], hf.ap, reads=[hf])

    cbs_cur = [None] * 4

    phases = []
    for l in range(n_layers):
        phases.append(("p1_%d" % l, lambda l=l: phase1(l)))
        phases.append(("p2_%d" % l, lambda l=l: phase2(l)))
        phases.append(("p3_%d" % l, lambda l=l: phase3(l)))
        phases.append(("p4_%d" % l, lambda l=l: phase4(l, l == n_layers - 1 and n_layers == 2)))
    for name, fn in phases:
        fn()
        if stop_phase is not None and stop_phase.replace("p1a", "p1") == name:
            break
    if dbg:
        new_phase()
        srcs = {"QA": QA, "KA": KA, "QB": QB, "KB": KB, "QC": QC, "KC": KC, "VS": VS, "GS": GS, "OT": OT, "XT": XT}
        for name, ap in dbg.items():
            if name.replace("dbg_", "") not in srcs:
                continue
            s = srcs[name.replace("dbg_", "")]
            P.DMA(ap, s)
    P.finalize()
    P.used_inputs = list(used_inputs.keys())
    return nc, P


def _tables(role):
    bf = ml_dtypes.bfloat16
    cb = np.zeros((128, NCBF), np.float32)
    cb[:, C_ID:C_ID + 128] = np.eye(128)
    cb[:, C_ONES:C_ONES + 128] = 1.0
    j = np.arange(128)[:, None]
    s_ = np.arange(128)[None, :]
    cb[:, C_NTRI:C_NTRI + 128] = np.where(j >= s_, -1.0, 0.0)
    cb[:, C_NONES:C_NONES + 128] = -1.0
    k = np.arange(128)[:, None]
    q = np.arange(128)[None, :]
    for a in range(4):
        for c in range(4):
            strict = np.where((a > c) | ((a == c) & (k >= q)), NEG, 0.0)
            nonstrict = np.where((a > c) | ((a == c) & (k > q)), NEG, 0.0)
            cb[:, C_CBC + a * 512 + c * 128:C_CBC + a * 512 + (c + 1) * 128] = strict
            cb[:, C_CBA + a * 512 + c * 128:C_CBA + a * 512 + (c + 1) * 128] = nonstrict
    cb[:, C_CBB:C_CBB + 128] = np.where(k > q, NEG, 0.0)
    cb[:, C_CBB + 128:C_CBB + 256] = np.where(k <= q, NEG, 0.0)
    cbf = cb.astype(bf)
    pbt = np.zeros((16, 16), np.float32)
    t1 = np.zeros((16, 16), np.float32)
    t0 = np.zeros((16, 16), np.float32)
    for own in range(16):
        for n in range(16):
            if role == 1 or own < 8:
                valid = n < own
            else:
                valid = 8 <= n < own
            pbt[own, n] = 0.0 if valid else -1e30
            t1[own, n] = -NEG if valid else 0.0
            t0[own, n] = NEG if valid else (0.0 if n == own else NEG)
    rows = np.zeros((18, NCTX), np.float32)
    pos = np.arange(NCTX)
    rows[np.arange(NCTX) // 256, np.arange(NCTX)] = 1.0
    if role == 0:
        rows[16, :HALF] = NEG
        rows[17, HALF:] = 1.0
        pos = np.where(pos >= HALF, pos - HALF, pos)
    else:
        rows[17, :] = 1.0
    rows_bf = rows.astype(bf)
    half = 32
    inv_freq = (np.float32(10000.0) ** (-np.arange(half, dtype=np.float32) / np.float32(half))).astype(np.float32)
    ang = pos.astype(np.float32)[:, None] * inv_freq[None, :]
    cos = np.cos(ang).astype(np.float32).T
    sin = np.sin(ang).astype(np.float32).T
    cosT = np.concatenate([cos, cos], 0)
    sinT = np.concatenate([-sin, sin], 0)
    rope = np.stack([cosT * np.float32(0.125), sinT * np.float32(0.125), cosT, sinT]).astype(np.float32)
    return cbf, pbt, t1, t0, rows_bf, rope


def _prep_inputs(inp):
    f = lambda a: np.ascontiguousarray(np.asarray(a, dtype=np.float32))
    x = f(inp["x"])
    mem = f(inp["mem"])
    cols = _w_in_cols()
    w_in = f(inp["w_in"])
    w_in_ext = np.zeros((2, D, NCOLX), np.float32)
    valid = cols >= 0
    w_in_ext[:, :, valid] = w_in[:, :, cols[valid]]
    w_proj = np.concatenate([f(inp["w_proj_a"]), f(inp["w_proj_b"]), f(inp["w_proj_c"])], axis=1)
    gains = np.zeros((128, 72), np.float32)
    for off, key in ((G_MIX, "norm_mix"), (G_CROSS, "norm_cross"), (G_MEM, "norm_mem"), (G_FFN, "norm_ffn")):
        g = f(inp[key])
        for l in range(2):
            gains[:, off + 8 * l:off + 8 * l + 8] = g[l].reshape(8, 128).T
    gains[:, G_FINAL:G_FINAL + 8] = f(inp["final_norm"]).reshape(8, 128).T
    sinks = f(inp["sinks"]).reshape(1, 16)
    shared = {
        "w_in_ext": w_in_ext, "w_proj": w_proj, "w_mix_out": f(inp["w_mix_out"]),
        "w_xq": f(inp["w_xq"]), "w_xkv": f(inp["w_xkv"]), "w_xo": f(inp["w_xo"]),
        "ffn_gate": f(inp["ffn_gate"]), "ffn_up": f(inp["ffn_up"]), "ffn_down": f(inp["ffn_down"]),
        "moe_router": f(inp["moe_router"]), "moe_gate": f(inp["moe_gate"]), "moe_up": f(inp["moe_up"]),
        "moe_down": f(inp["moe_down"]),
    }
    in_maps = []
    tabs = [_tables(0), _tables(1)]
    for c in range(8):
        b, role = c // 2, c % 2
        cbf, pbt, t1, t0, rows_bf, rope = tabs[role]
        if role == 1:
            ctx = x[b]
        else:
            ctx = np.concatenate([np.zeros((HALF, D), np.float32), x[b, :HALF]], axis=0)
        cf = np.zeros((128, NCF), np.float32)
        cf[:, F_ID:F_ID + 128] = np.eye(128, dtype=np.float32)
        own_of_tile = np.arange(32) // 2
        cf[:, F_PB:F_PB + 512] = pbt[own_of_tile].reshape(1, 512)
        cf[:, F_T1:F_T1 + 512] = t1[own_of_tile].reshape(1, 512)
        cf[:, F_T0:F_T0 + 512] = t0[own_of_tile].reshape(1, 512)
        cf[:, F_ONE] = 1.0
        cf[:, F_ONES32:F_ONES32 + 128] = 1.0
        cf[:, F_GAIN:F_GAIN + 72] = gains
        cf[:, F_SINK:F_SINK + 16] = sinks
        cf[:, F_EPS] = EPS
        m = dict(shared)
        m.update({"xT": np.ascontiguousarray(ctx.T), "memT": np.ascontiguousarray(mem[b].T),
                  "cbf": cbf, "cf32": cf, "rows_bf": rows_bf, "rope": rope})
        in_maps.append(m)
    return in_maps


_NC_CACHE = {}


def kernel(**inputs):
    in_maps = _prep_inputs(inputs)
    if "nc" not in _NC_CACHE:
        _NC_CACHE["nc"] = build()[0]
    nc = _NC_CACHE["nc"]
    res = run_bass_kernel_spmd(nc, in_maps, core_ids=list(range(8)))
    out = np.zeros((4, SEQ, D), np.float32)
    for c in range(8):
        b, role = c // 2, c % 2
        o = np.asarray(res.results[c]["outT"]).T
        if role == 0:
            out[b, :HALF] = o
        else:
            out[b, HALF:] = o
    return out
```
